# Optimizing a Trainium2 kernel written in Bass

```python
import jax, jax.numpy as jnp
from jax import lax
import numpy as np

D_MODEL = 1024
BATCH = 2
SEQ = 16384
DEPTH = 4

GRID_W = 64
CTX_LEN = 256
HEAD_DIM = 64
ATTN_SCALE = HEAD_DIM ** -0.5
CONV_A_CH = D_MODEL // 2
CONV_A_WIDTH = 31
CONV_B_CH = D_MODEL // 2
CONV_B_WIDTH = 3
CONV_SPLITS = [CONV_A_CH, 2 * CONV_A_CH, 2 * CONV_A_CH + CONV_B_CH, 2 * CONV_A_CH + 2 * CONV_B_CH]
CONV_IN_COLS = 2 * CONV_A_CH + 3 * CONV_B_CH
CONV_OUT_ROWS = CONV_A_CH + CONV_B_CH
GQA_Q_HEADS = (D_MODEL // 2) // HEAD_DIM
GQA_KV_HEADS = GQA_Q_HEADS // 4
NA_HEADS = (D_MODEL // 2) // HEAD_DIM
NA_WIN_ROWS = 8
NA_WIN_COLS = 16
Q_BLOCK = 128
ROPE_THETA = 10000.0
ROPE_AXIS_DIM = HEAD_DIM // 2
Q_COLS_C = GQA_Q_HEADS * HEAD_DIM
KV_COLS_C = GQA_KV_HEADS * HEAD_DIM
COLS_D = NA_HEADS * HEAD_DIM
ATTN_SPLITS = [Q_COLS_C, Q_COLS_C + KV_COLS_C, Q_COLS_C + 2 * KV_COLS_C,
               Q_COLS_C + 2 * KV_COLS_C + COLS_D, Q_COLS_C + 2 * KV_COLS_C + 2 * COLS_D]
ATTN_IN_COLS = Q_COLS_C + 2 * KV_COLS_C + 3 * COLS_D
ATTN_OUT_ROWS = Q_COLS_C + COLS_D
N_GROUPS = 4
EXPERTS_PER_GROUP = 8
N_EXPERTS = N_GROUPS * EXPERTS_PER_GROUP
TOP_K_INNER = 2
EXPERT_HIDDEN = D_MODEL // 2
DISPATCH_BLOCK = 128
DEEPNORM_ALPHA = (2 * DEPTH) ** 0.25
DEEPNORM_BETA = (8 * DEPTH) ** -0.25
N_EVEN = (DEPTH + 1) // 2
N_ODD = DEPTH // 2
LN_EPS = 1e-5
RMS_EPS = 1e-6

kernel_name = 'hybrid_dit_conv_attn_hmoe'


def layer_norm(x, g, b):
    xf = x.astype(jnp.float32)
    mu = jnp.mean(xf, axis=-1, keepdims=True)
    var = jnp.mean(jnp.square(xf - mu), axis=-1, keepdims=True)
    return ((xf - mu) * lax.rsqrt(var + LN_EPS) * g + b).astype(x.dtype)


def rms_norm(x, g):
    xf = x.astype(jnp.float32)
    return (xf * lax.rsqrt(jnp.mean(xf * xf, axis=-1, keepdims=True) + RMS_EPS) * g).astype(x.dtype)


def ada_split(cond, w, b):
    return jnp.split(jax.nn.silu(cond) @ w + b, 6, axis=-1)


def depthwise_conv(h, w):
    k = w.shape[0]
    return lax.conv_general_dilated(h, w[:, None, :].astype(h.dtype), (1,), [(k // 2, k // 2)],
                                    dimension_numbers=('NWC', 'WIO', 'NWC'),
                                    feature_group_count=h.shape[-1])


def split_heads(z, n):
    b, l, _ = z.shape
    return z.reshape(b, l, n, HEAD_DIM).transpose(0, 2, 1, 3)


def merge_heads(o):
    b, n, l, d = o.shape
    return o.transpose(0, 2, 1, 3).reshape(b, l, n * d)


def axial_rope_tables(n_tokens):
    t = jnp.arange(n_tokens)
    row = (t // GRID_W).astype(jnp.float32)
    col = (t % GRID_W).astype(jnp.float32)
    inv = ROPE_THETA ** (-jnp.arange(0, ROPE_AXIS_DIM, 2, dtype=jnp.float32) / ROPE_AXIS_DIM)
    ar = row[:, None] * inv
    ac = col[:, None] * inv
    return jnp.cos(ar), jnp.sin(ar), jnp.cos(ac), jnp.sin(ac)


def rotate(x, cos, sin):
    x1, x2 = jnp.split(x, 2, axis=-1)
    return jnp.concatenate([x1 * cos - x2 * sin, x1 * sin + x2 * cos], axis=-1)


def apply_axial_rope(x, tables):
    cr, sr, cc, sc = [t.astype(x.dtype) for t in tables]
    xr, xc = jnp.split(x, 2, axis=-1)
    return jnp.concatenate([rotate(xr, cr, sr), rotate(xc, cc, sc)], axis=-1)


def softmax_attention(q, k, v):
    s = jnp.einsum('bkgqd,bksd->bkgqs', q, k, preferred_element_type=jnp.float32) * ATTN_SCALE
    p = jax.nn.softmax(s, axis=-1).astype(v.dtype)
    return jnp.einsum('bkgqs,bksd->bkgqd', p, v)


def gqa_latent(q, k_all, v_all):
    b, hk, g, s, d = q.shape
    nb = s // Q_BLOCK
    qb = jnp.moveaxis(q.reshape(b, hk, g, nb, Q_BLOCK, d), 3, 0)
    o = lax.map(lambda qi: softmax_attention(qi, k_all, v_all), qb)
    return jnp.moveaxis(o, 0, 3).reshape(b, hk * g, s, d)


def neighbourhood_latent(q, k, v, k_ctx, v_ctx, rel_bias):
    b, h, s, d = q.shape
    rows = s // GRID_W
    kr = min(NA_WIN_ROWS, rows)
    n_ctx = k_ctx.shape[2]
    qg = jnp.moveaxis(q.reshape(b, h, rows, GRID_W, d), 2, 0)
    kg = k.reshape(b, h, rows, GRID_W, d)
    vg = v.reshape(b, h, rows, GRID_W, d)
    qcol = jnp.arange(GRID_W)
    cstart = jnp.clip(qcol - NA_WIN_COLS // 2, 0, GRID_W - NA_WIN_COLS)
    in_win = (qcol[None, :] >= cstart[:, None]) & (qcol[None, :] < cstart[:, None] + NA_WIN_COLS)
    col_idx = jnp.clip(qcol[None, :] - qcol[:, None] + NA_WIN_COLS - 1, 0, 2 * NA_WIN_COLS - 2)
    bias_cols = rel_bias[:, :, col_idx]

    def one_row(args):
        qr, r = args
        rs = jnp.clip(r - kr // 2, 0, rows - kr)
        kb = lax.dynamic_slice_in_dim(kg, rs, kr, axis=2)
        vb = lax.dynamic_slice_in_dim(vg, rs, kr, axis=2)
        s_lat = jnp.einsum('bhqd,bhrkd->bhqrk', qr, kb, preferred_element_type=jnp.float32) * ATTN_SCALE
        row_idx = rs + jnp.arange(kr) - r + NA_WIN_ROWS - 1
        bias = jnp.take(bias_cols, row_idx, axis=1).transpose(0, 2, 1, 3).astype(jnp.float32)
        s_lat = jnp.where(in_win[:, None, :], s_lat + bias, -jnp.inf).reshape(b, h, GRID_W, kr * GRID_W)
        s_ctx = jnp.einsum('bhqd,bhcd->bhqc', qr, k_ctx, preferred_element_type=jnp.float32) * ATTN_SCALE
        p = jax.nn.softmax(jnp.concatenate([s_ctx, s_lat], axis=-1), axis=-1).astype(v.dtype)
        p_ctx = p[..., :n_ctx]
        p_lat = p[..., n_ctx:].reshape(b, h, GRID_W, kr, GRID_W)
        return (jnp.einsum('bhqc,bhcd->bhqd', p_ctx, v_ctx)
                + jnp.einsum('bhqrk,bhrkd->bhqd', p_lat, vb))

    o = lax.map(one_row, (qg, jnp.arange(rows)))
    return jnp.moveaxis(o, 0, 2).reshape(b, h, s, d)


def conv_mixers(h, w_in, w_out, a_dw_w, a_dw_b, a_ln_g, a_ln_b, b_dw_w):
    z = h @ w_in
    a_val, a_gate, b_gate, c_gate, b_val = jnp.split(z, CONV_SPLITS, axis=-1)
    ya = a_val * jax.nn.sigmoid(a_gate)
    ya = depthwise_conv(ya, a_dw_w) + a_dw_b
    ya = jax.nn.silu(layer_norm(ya, a_ln_g, a_ln_b))
    yb = b_gate * depthwise_conv(c_gate * b_val, b_dw_w)
    return jnp.concatenate([ya, yb], axis=-1) @ w_out


def attn_mixers(h_lat, h_ctx, w_in, w_out, q_g, k_g, rel_bias, with_ctx_out):
    b, s, _ = h_lat.shape
    n_ctx = h_ctx.shape[1]
    group = GQA_Q_HEADS // GQA_KV_HEADS
    ql_c, kl_c, vl_c, ql_d, kl_d, vl_d = jnp.split(h_lat @ w_in, ATTN_SPLITS, axis=-1)
    qc_c, kc_c, vc_c, qc_d, kc_d, vc_d = jnp.split(h_ctx @ w_in, ATTN_SPLITS, axis=-1)
    tables = axial_rope_tables(s)
    q_lat = apply_axial_rope(rms_norm(split_heads(ql_c, GQA_Q_HEADS), q_g), tables)
    q_lat = q_lat.reshape(b, GQA_KV_HEADS, group, s, HEAD_DIM)
    k_lat = apply_axial_rope(rms_norm(split_heads(kl_c, GQA_KV_HEADS), k_g), tables)
    v_lat = split_heads(vl_c, GQA_KV_HEADS)
    k_ctx = rms_norm(split_heads(kc_c, GQA_KV_HEADS), k_g)
    v_ctx = split_heads(vc_c, GQA_KV_HEADS)
    o_c = gqa_latent(q_lat, jnp.concatenate([k_ctx, k_lat], axis=2), jnp.concatenate([v_ctx, v_lat], axis=2))
    kd_ctx = split_heads(kc_d, NA_HEADS)
    vd_ctx = split_heads(vc_d, NA_HEADS)
    o_d = neighbourhood_latent(split_heads(ql_d, NA_HEADS), split_heads(kl_d, NA_HEADS),
                               split_heads(vl_d, NA_HEADS), kd_ctx, vd_ctx, rel_bias)
    y_lat = jnp.concatenate([merge_heads(o_c), merge_heads(o_d)], axis=-1) @ w_out
    if not with_ctx_out:
        return y_lat, None
    q_ctx = rms_norm(split_heads(qc_c, GQA_Q_HEADS), q_g).reshape(b, GQA_KV_HEADS, group, n_ctx, HEAD_DIM)
    oc_c = softmax_attention(q_ctx, k_ctx, v_ctx).reshape(b, GQA_Q_HEADS, n_ctx, HEAD_DIM)
    oc_d = softmax_attention(split_heads(qc_d, NA_HEADS)[:, :, None], kd_ctx, vd_ctx)[:, :, 0]
    y_ctx = jnp.concatenate([merge_heads(oc_c), merge_heads(oc_d)], axis=-1) @ w_out
    return y_lat, y_ctx


def grouped_experts(x, expert, gate, w_gate, w_up, w_down):
    n, d = x.shape
    k = expert.shape[1]
    m = n * k
    flat_e = expert.reshape(m)
    flat_tok = jnp.arange(m) // k
    flat_w = gate.reshape(m)
    order = jnp.argsort(flat_e)
    se = flat_e[order]
    counts = jnp.bincount(flat_e, length=N_EXPERTS)
    padded = (counts + DISPATCH_BLOCK - 1) // DISPATCH_BLOCK * DISPATCH_BLOCK
    pad_end = jnp.cumsum(padded)
    pad_start = pad_end - padded
    start = jnp.cumsum(counts) - counts
    dest = pad_start[se] + jnp.arange(m) - start[se]
    n_blocks = -(-m // DISPATCH_BLOCK) + N_EXPERTS
    cap = n_blocks * DISPATCH_BLOCK
    slot_tok = jnp.full((cap,), n, jnp.int32).at[dest].set(flat_tok[order])
    slot_w = jnp.zeros((cap,), x.dtype).at[dest].set(flat_w[order])
    xs = jnp.concatenate([x, jnp.zeros((1, d), x.dtype)], axis=0)[slot_tok].reshape(n_blocks, DISPATCH_BLOCK, d)
    block_e = jnp.minimum(jnp.searchsorted(pad_end, jnp.arange(n_blocks) * DISPATCH_BLOCK, side='right'),
                          N_EXPERTS - 1)

    def run(args):
        xb, e = args
        return (jax.nn.silu(xb @ w_gate[e]) * (xb @ w_up[e])) @ w_down[e]

    ys = lax.map(run, (xs, block_e)).reshape(cap, d)
    return jnp.zeros((n + 1, d), x.dtype).at[slot_tok].add(ys * slot_w[:, None])[:n]


def hier_moe(tokens, w1, b1, w2, b2, w_gate, w_up, w_down):
    n = tokens.shape[0]
    p1 = jax.nn.softmax((tokens @ w1 + b1).astype(jnp.float32), axis=-1)
    p_grp, grp = lax.top_k(p1, 1)
    lg2 = (tokens @ w2 + b2).astype(jnp.float32).reshape(n, N_GROUPS, EXPERTS_PER_GROUP)
    lg2 = jnp.take_along_axis(lg2, grp[:, :, None], axis=1)[:, 0]
    top_val, top_idx = lax.top_k(lg2, TOP_K_INNER)
    gate = p_grp * jax.nn.softmax(top_val, axis=-1)
    expert = grp * EXPERTS_PER_GROUP + top_idx
    return grouped_experts(tokens, expert, gate.astype(tokens.dtype), w_gate, w_up, w_down)


def setup_inputs(seed: int = 0) -> dict:
    key = jax.random.key(seed)
    keys = jax.random.split(key, 32)
    counter = [0]
    D = D_MODEL

    def nrm(shape, scale):
        kk = keys[counter[0]]
        counter[0] += 1
        return jax.random.normal(kk, shape, jnp.float32) * scale

    return {
        'x': nrm((BATCH, SEQ, D), 1.0),
        'c': nrm((BATCH, D), 1.0),
        'ctx': nrm((BATCH, CTX_LEN, D), 1.0),
        'c_ctx': nrm((D,), 1.0),
        'ada_w': nrm((DEPTH, D, 6 * D), 0.5 * D ** -0.5),
        'ada_b': nrm((DEPTH, 6 * D), 0.02),
        'ln1_g': 1.0 + nrm((DEPTH, D), 0.02),
        'ln1_b': nrm((DEPTH, D), 0.02),
        'ln2_g': 1.0 + nrm((DEPTH, D), 0.02),
        'ln2_b': nrm((DEPTH, D), 0.02),
        'conv_w_in': nrm((N_EVEN, D, CONV_IN_COLS), D ** -0.5),
        'conv_w_out': nrm((N_EVEN, CONV_OUT_ROWS, D), DEEPNORM_BETA * CONV_OUT_ROWS ** -0.5),
        'conv_a_dw_w': nrm((N_EVEN, CONV_A_WIDTH, CONV_A_CH), CONV_A_WIDTH ** -0.5),
        'conv_a_dw_b': nrm((N_EVEN, CONV_A_CH), 0.02),
        'conv_a_ln_g': 1.0 + nrm((N_EVEN, CONV_A_CH), 0.02),
        'conv_a_ln_b': nrm((N_EVEN, CONV_A_CH), 0.02),
        'conv_b_dw_w': nrm((N_EVEN, CONV_B_WIDTH, CONV_B_CH), CONV_B_WIDTH ** -0.5),
        'attn_w_in': nrm((N_ODD, D, ATTN_IN_COLS), D ** -0.5),
        'attn_w_out': nrm((N_ODD, ATTN_OUT_ROWS, D), DEEPNORM_BETA * ATTN_OUT_ROWS ** -0.5),
        'q_norm_g': 1.0 + nrm((N_ODD, HEAD_DIM), 0.02),
        'k_norm_g': 1.0 + nrm((N_ODD, HEAD_DIM), 0.02),
        'na_rel_bias': nrm((N_ODD, NA_HEADS, 2 * NA_WIN_ROWS - 1, 2 * NA_WIN_COLS - 1), 0.05),
        'router_w1': nrm((DEPTH, D, N_GROUPS), D ** -0.5),
        'router_b1': nrm((DEPTH, N_GROUPS), 0.01),
        'router_w2': nrm((DEPTH, D, N_EXPERTS), D ** -0.5),
        'router_b2': nrm((DEPTH, N_EXPERTS), 0.01),
        'expert_w_gate': nrm((DEPTH, N_EXPERTS, D, EXPERT_HIDDEN), D ** -0.5),
        'expert_w_up': nrm((DEPTH, N_EXPERTS, D, EXPERT_HIDDEN), D ** -0.5),
        'expert_w_down': nrm((DEPTH, N_EXPERTS, EXPERT_HIDDEN, D), DEEPNORM_BETA * EXPERT_HIDDEN ** -0.5),
    }


def reference(x, c, ctx, c_ctx, ada_w, ada_b, ln1_g, ln1_b, ln2_g, ln2_b,
              conv_w_in, conv_w_out, conv_a_dw_w, conv_a_dw_b, conv_a_ln_g, conv_a_ln_b, conv_b_dw_w,
              attn_w_in, attn_w_out, q_norm_g, k_norm_g, na_rel_bias,
              router_w1, router_b1, router_w2, router_b2, expert_w_gate, expert_w_up, expert_w_down):
    d = x.shape[-1]
    x_lat, x_ctx = x, ctx
    for i in range(DEPTH):
        last = i == DEPTH - 1
        odd = i % 2 == 1
        j = i // 2
        sh1, sc1, g1, sh2, sc2, g2 = [m[:, None, :] for m in ada_split(c, ada_w[i], ada_b[i])]
        csh1, csc1, cg1, csh2, csc2, cg2 = ada_split(c_ctx, ada_w[i], ada_b[i])
        moe_w = (router_w1[i], router_b1[i], router_w2[i], router_b2[i],
                 expert_w_gate[i], expert_w_up[i], expert_w_down[i])
        h_lat = x_lat * (1.0 + sc1) + sh1
        h_ctx = x_ctx * (1.0 + csc1) + csh1 if (odd or not last) else None
        if odd:
            y_lat, y_ctx = attn_mixers(h_lat, h_ctx, attn_w_in[j], attn_w_out[j], q_norm_g[j], k_norm_g[j],
                                       na_rel_bias[j], not last)
        else:
            conv_w = (conv_w_in[j], conv_w_out[j], conv_a_dw_w[j], conv_a_dw_b[j],
                      conv_a_ln_g[j], conv_a_ln_b[j], conv_b_dw_w[j])
            y_lat = conv_mixers(h_lat, *conv_w)
            y_ctx = None if last else conv_mixers(h_ctx, *conv_w)
        x_lat = layer_norm(DEEPNORM_ALPHA * x_lat + g1 * y_lat, ln1_g[i], ln1_b[i])
        h_lat = x_lat * (1.0 + sc2) + sh2
        if last:
            y = hier_moe(h_lat.reshape(-1, d), *moe_w).reshape(x_lat.shape)
            x_lat = layer_norm(DEEPNORM_ALPHA * x_lat + g2 * y, ln2_g[i], ln2_b[i])
        else:
            x_ctx = layer_norm(DEEPNORM_ALPHA * x_ctx + cg1 * y_ctx, ln1_g[i], ln1_b[i])
            h_ctx = x_ctx * (1.0 + csc2) + csh2
            n_lat = h_lat.shape[0] * h_lat.shape[1]
            y = hier_moe(jnp.concatenate([h_lat.reshape(-1, d), h_ctx.reshape(-1, d)], axis=0), *moe_w)
            x_lat = layer_norm(DEEPNORM_ALPHA * x_lat + g2 * y[:n_lat].reshape(x_lat.shape), ln2_g[i], ln2_b[i])
            x_ctx = layer_norm(DEEPNORM_ALPHA * x_ctx + cg2 * y[n_lat:].reshape(x_ctx.shape), ln2_g[i], ln2_b[i])
    return x_lat
```

```python
import numpy as np
from contextlib import ExitStack, contextmanager
import concourse.bass as bass
import concourse.mybir as mybir
from concourse.bass_utils import run_bass_kernel_spmd

F32 = mybir.dt.float32
BF16 = mybir.dt.bfloat16
AF = mybir.ActivationFunctionType
ALU = mybir.AluOpType
AX = mybir.AxisListType

SEM_LIMIT = 30000
D = 1024
NLAT = 16384
NCORES = 2
QPB = 1
NCTX = 256
NTOK = NLAT + NCTX
ALPHA = float((2 * 4) ** 0.25)
LN_EPS = 1e-5
RMS_EPS = 1e-6
NEXP = 32
NEG = -30000.0
DEBUG = {}


class Sem:
    def __init__(self, mk, name):
        self.mk, self.name, self.cur, self.val, self.n = mk, name, None, 0, 0

    def bump(self, inc):
        if self.cur is None or self.val + inc > SEM_LIMIT:
            self.cur = self.mk.es0.enter_context(self.mk.nc.semaphore("%s_%d" % (self.name, self.n)))
            self.mk.nsem += 1
            self.n += 1
            self.val = 0
        self.val += inc
        return (self.cur, self.val)


class T:
    __slots__ = ("w", "r", "dsem", "name", "base")

    def __init__(self, name=""):
        self.w, self.r, self.dsem, self.name, self.base = [], [], None, name, []


class Tile:
    def __init__(self, t, tr):
        self.t, self.tr = t, tr

    def __getitem__(self, idx):
        return self.t[idx]


class EngState:
    def __init__(self, mk, name):
        self.name = name
        self.eng = getattr(mk.nc, name)
        self.sem = Sem(mk, "s_" + name)
        self.waited = {}
        self.ninst = 0


def _compact(lst):
    d = {}
    for t_ in lst:
        k = id(t_[0])
        if k not in d or d[k][1] < t_[1]:
            d[k] = t_
    return list(d.values())


class Pool:
    def __init__(self, tiles):
        self.tiles, self.i = tiles, 0

    def next(self):
        t = self.tiles[self.i % len(self.tiles)]
        self.i += 1
        return t


class MK:
    def __init__(self, nc, es):
        self.nc, self.es, self.es0 = nc, es, es
        self.nsem = 0
        self.E = {n: EngState(self, n) for n in ["tensor", "vector", "scalar", "gpsimd", "sync"]}
        self.dsems = []
        self.uid = 0

    @contextmanager
    def scope(self):
        old = self.es
        with ExitStack() as es2:
            self.es = es2
            try:
                yield
                self.barrier()
            finally:
                self.es = old

    def sb(self, name, shape, dt):
        self.uid += 1
        t = self.es.enter_context(self.nc.sbuf_tensor("%s_%d" % (name, self.uid), list(shape), dt))
        return Tile(t, T(name))

    def ps(self, name, shape, dt=F32):
        self.uid += 1
        t = self.es.enter_context(self.nc.psum_tensor("%s_%d" % (name, self.uid), list(shape), dt))
        return Tile(t, T(name))

    def pool(self, name, shape, dt, n, psum=False):
        return Pool([(self.ps if psum else self.sb)("%s%d" % (name, i), shape, dt) for i in range(n)])

    def _wait(self, es, tok):
        sem, val = tok[0], tok[1]
        key = id(sem)
        if es.waited.get(key, 0) >= val:
            return
        es.waited[key] = val
        es.eng.wait_ge(sem, val)

    def _deps(self, es, R, W, join, skip_same):
        for tr in R:
            for tok in tr.w:
                if not (skip_same and tok[2] is es):
                    self._wait(es, tok)
        for tr in W:
            for tok in (tr.base if join else tr.w):
                if not (skip_same and tok[2] is es):
                    self._wait(es, tok)
            for tok in tr.r:
                if not (skip_same and tok[2] is es):
                    self._wait(es, tok)

    def _record(self, tok3, R, W, join):
        for tr in R:
            tr.r.append(tok3)
            if len(tr.r) > 12:
                tr.r = _compact(tr.r)
        for tr in W:
            if join:
                tr.w.append(tok3)
                if len(tr.w) > 12:
                    tr.w = _compact(tr.w)
            else:
                tr.w = [tok3]
                tr.base = [tok3]
                tr.r = []

    @staticmethod
    def _trs(lst):
        return [x.tr if isinstance(x, Tile) else x for x in lst]

    def do(self, eng, fn, R=(), W=(), join=False):
        es = self.E[eng]
        R, W = self._trs(R), self._trs(W)
        self._deps(es, R, W, join, skip_same=(eng == "tensor"))
        ins = fn(es.eng)
        sem, val = es.sem.bump(1)
        ins.then_inc(sem, 1)
        es.ninst += 1
        self._record((sem, val, es), R, W, join)
        return ins

    def dma(self, q, out, in_, R=(), W=(), join=False, **kw):
        es = self.E[q]
        R, W = self._trs(R), self._trs(W)
        self._deps(es, R, W, join, skip_same=False)
        owner = (W + R)[0]
        if owner.dsem is None:
            owner.dsem = Sem(self, "d%d" % self.uid)
            self.uid += 1
            self.dsems.append(owner.dsem)
        ins = es.eng.dma_start(out=out, in_=in_, **kw)
        sem, val = owner.dsem.bump(16)
        ins.then_inc(sem, 16)
        es.ninst += 1
        self._record((sem, val, None), R, W, join)
        return ins

    def drain_dma(self, engs):
        for ds in self.dsems:
            if ds.cur is not None:
                for e in engs:
                    self._wait(self.E[e], (ds.cur, ds.val))

    def barrier(self, engs=("tensor", "vector", "scalar", "gpsimd", "sync")):
        for e in engs:
            es = self.E[e]
            for o in self.E.values():
                if o.sem.cur is not None and o is not es:
                    self._wait(es, (o.sem.cur, o.sem.val))
        self.drain_dma(engs)


class Ctx:
    pass


def setup_common(mk, nc, kind):
    g = Ctx()
    g.mk, g.nc = mk, nc

    def din(name, shape, dt=F32):
        return nc.dram_tensor(name, list(shape), dt, kind="ExternalInput").ap()

    g.din = din
    g.xl = din("xl", [NLAT, D])
    g.xc = din("xc", [NCTX, D])
    g.condT = din("condT", [128, 16])
    g.ada_w = din("ada_w", [D, 6 * D])
    g.ada_b = din("ada_b", [1, 6 * D])
    g.ln = din("ln", [4, D])
    g.identd = din("ident", [128, 128])
    g.rw = din("rw", [D, 36])
    g.rb = din("rb", [1, 36])
    g.wg = din("wg", [NEXP * D, 512])
    g.wu = din("wu", [NEXP * D, 512])
    g.wd = din("wd", [NEXP * 512, D])
    g.ol = nc.dram_tensor("ol", [NLAT, D], F32, kind="ExternalOutput").ap()
    g.oc = nc.dram_tensor("oc", [NCTX, D], F32, kind="ExternalOutput").ap()
    g.x1 = nc.dram_tensor("x1s", [NTOK, D], F32, kind=("ExternalOutput" if DEBUG.get("x1out") else "Internal")).ap()

    g.ident = mk.sb("ident", [128, 128], F32)
    g.ones = mk.sb("ones", [128, 128], F32)
    g.modc = mk.sb("modc", [128, 2 * 6 * 8], F32)
    mk.dma("sync", g.ident[:], g.identd[:, :], W=[g.ident])
    mk.do("vector", lambda e: e.memset(g.ones[:], 1.0), W=[g.ones])
    g.pT = mk.pool("pT", [128, 512], F32, 2, psum=True)
    g.pM = mk.pool("pM", [128, 512], F32, 4, psum=True)
    g.pY = mk.pool("pY", [128, 512], F32, 2, psum=True)
    g.xt = mk.pool("xt", [128, D], F32, 4)
    return g


def dbg(g, name, tile, shape, dt=F32):
    if not DEBUG.get("dump"):
        return
    o = g.nc.dram_tensor("dbg_" + name, list(shape), dt, kind="ExternalOutput").ap()
    g.mk.dma("sync", o, tile[:], R=[tile])


def tok_src(g, s, t0, n):
    return (g.xl if s == 0 else g.xc)[t0:t0 + n, :]


def mcol(g, s, v, c):
    i = (s * 6 + v) * 8 + c
    return g.modc[:, i:i + 1]


def phase_ada(g, gB):
    mk = g.mk
    with mk.scope():
        ct = mk.sb("condT", [128, 16], F32)
        sc = mk.sb("silu", [128, 16], F32)
        L = mk.sb("L", [128, 16, 128], F32)
        awp = mk.pool("aw", [128, 8, 512], F32, 2)
        abp = mk.pool("ab", [128, 512], F32, 2)
        tmp = mk.pool("adatmp", [128, 512], F32, 2)
        junk = mk.sb("adajunk", [128, 128], F32)
        mk.dma("sync", ct[:], g.condT[:, :], W=[ct])
        mk.do("scalar", lambda e: e.activation(out=sc[:], in_=ct[:], func=AF.Silu), R=[ct], W=[sc])
        for i in range(16):
            mk.do("vector", lambda e: e.tensor_scalar(out=L[:, i, :], in0=g.ones[:], scalar1=sc[:, i:i + 1],
                                                       scalar2=None, op0=ALU.mult), R=[g.ones, sc], W=[L], join=True)
        dbg(g, "sc", sc, [128, 16])
        dbg(g, "L", L, [128, 16, 128])
        awr = g.ada_w.rearrange("(k p) n -> p k n", p=128)
        for jb in range(12):
            aw = awp.next()
            ab = abp.next()
            mk.dma("sync", aw[:], awr[:, :, jb * 512:(jb + 1) * 512], W=[aw])
            mk.dma("sync", ab[:], g.ada_b[0:1, jb * 512:(jb + 1) * 512].partition_broadcast(128), W=[ab])
            v, half = jb // 2, jb % 2
            if jb == 4:
                dbg(g, "ab4", ab, [128, 512])
                dbg(g, "aw4", aw, [128, 8, 512])
            for s in range(2):
                p = g.pM.next()
                for k in range(8):
                    mk.do("tensor", lambda e: e.matmul(p[:], L[:, k * 2 + s, :], aw[:, k, :], start=(k == 0), stop=(k == 7)),
                          R=[L, aw], W=[p])
                if v in (2, 5):
                    dst = gB[(v, s)]
                    mk.do("vector", lambda e: e.tensor_tensor(out=dst[:, half * 512:(half + 1) * 512], in0=p[:], in1=ab[:], op=ALU.add),
                          R=[p, ab], W=[dst], join=True)
                else:
                    t = tmp.next()
                    mk.do("vector", lambda e: e.tensor_tensor(out=t[:], in0=p[:], in1=ab[:], op=ALU.add), R=[p, ab], W=[t])
                    for cc in range(4):
                        mk.do("vector", lambda e: e.tensor_tensor(out=junk[:], in0=t[:, cc * 128:(cc + 1) * 128], in1=g.ident[:], op=ALU.mult),
                              R=[t, g.ident], W=[junk])
                        i = (s * 6 + v) * 8 + half * 4 + cc
                        mk.do("vector", lambda e: e.reduce_sum(out=g.modc[:, i:i + 1], in_=junk[:], axis=AX.X),
                              R=[junk], W=[g.modc], join=True)
        for s in range(2):
            for v in (1, 4):
                i = (s * 6 + v) * 8
                mk.do("vector", lambda e: e.tensor_scalar_add(out=g.modc[:, i:i + 8], in0=g.modc[:, i:i + 8], scalar1=1.0),
                      R=[g.modc], W=[g.modc])


def load_bcast(g, dst, row_ap):
    g.mk.dma("sync", dst[:], row_ap.partition_broadcast(128), W=[dst])


def load_hT(g, s, t0, n, v_sh, v_sc, out_bf=None, out_f=None, src=None, f_eng="vector"):
    mk = g.mk
    nsub = (n + 127) // 128
    xts = []
    for j in range(nsub):
        r = min(128, n - j * 128)
        xt = g.xt.next()
        sap = src[j * 128:j * 128 + r, :] if src is not None else tok_src(g, s, t0 + j * 128, r)
        mk.dma("sync", xt[0:r, :], sap, W=[xt])
        xts.append((xt, r))
    for c in range(8):
        p = g.pT.next()
        for j, (xt, r) in enumerate(xts):
            mk.do("tensor", lambda e: e.transpose(out=p[:, j * 128:j * 128 + r], in_=xt[0:r, c * 128:(c + 1) * 128], identity=g.ident[0:r, 0:r]),
                  R=[xt, g.ident], W=[p], join=(j > 0))
        if out_bf is not None:
            mk.do("scalar", lambda e: e.activation(out=out_bf[:, c, 0:n], in_=p[:, 0:n], func=AF.Identity,
                                                   bias=mcol(g, s, v_sh, c), scale=mcol(g, s, v_sc, c)),
                  R=[p, g.modc], W=[out_bf], join=True)
        if out_f is not None:
            mk.do(f_eng, lambda e: e.tensor_scalar(out=out_f[:, c, 0:n], in0=p[:, 0:n], scalar1=mcol(g, s, v_sc, c),
                                                   scalar2=mcol(g, s, v_sh, c), op0=ALU.mult, op1=ALU.add),
                  R=[p, g.modc], W=[out_f], join=True)


def ln_epilogue(g, yh, yR, xt, gBt, lnG, lnB, dst_ap, tp):
    mk = g.mk
    r = tp["r"].next()
    for h in range(2):
        mk.do("vector", lambda e: e.tensor_tensor(out=r[:, h * 512:(h + 1) * 512], in0=yh[h], in1=gBt[:, h * 512:(h + 1) * 512], op=ALU.mult),
              R=yR + [gBt], W=[r], join=(h == 1))
    r2 = tp["r2"].next()
    mk.do("vector", lambda e: e.scalar_tensor_tensor(out=r2[:], in0=xt[:], scalar=ALPHA, in1=r[:], op0=ALU.mult, op1=ALU.add),
          R=[xt, r], W=[r2])
    st = tp["st"].next()
    mk.do("gpsimd", lambda e: e.memset(st[:], 0.0), W=[st])
    mk.do("scalar", lambda e: e.activation(out=r[:], in_=r2[:], func=AF.Identity, accum_out=st[:, 0:1]), R=[r2], W=[r, st])
    mk.do("scalar", lambda e: e.activation(out=r[:], in_=r2[:], func=AF.Square, accum_out=st[:, 1:2]), R=[r2], W=[r, st])
    mk.do("vector", lambda e: e.tensor_scalar(out=st[:, 2:4], in0=st[:, 0:2], scalar1=1.0 / D, scalar2=None, op0=ALU.mult), R=[st], W=[st])
    mk.do("vector", lambda e: e.tensor_tensor(out=st[:, 4:5], in0=st[:, 2:3], in1=st[:, 2:3], op=ALU.mult), R=[st], W=[st])
    mk.do("vector", lambda e: e.tensor_tensor(out=st[:, 3:4], in0=st[:, 3:4], in1=st[:, 4:5], op=ALU.subtract), R=[st], W=[st])
    mk.do("scalar", lambda e: e.activation(out=st[:, 3:4], in_=st[:, 3:4], func=AF.Sqrt, bias=LN_EPS, scale=1.0), R=[st], W=[st])
    mk.do("vector", lambda e: e.reciprocal(out=st[:, 3:4], in_=st[:, 3:4]), R=[st], W=[st])
    mk.do("vector", lambda e: e.tensor_scalar(out=r[:], in0=r2[:], scalar1=st[:, 2:3], scalar2=st[:, 3:4], op0=ALU.subtract, op1=ALU.mult),
          R=[r2, st], W=[r])
    mk.do("gpsimd", lambda e: e.tensor_tensor(out=r2[:], in0=r[:], in1=lnG[:], op=ALU.mult), R=[r, lnG], W=[r2])
    mk.do("gpsimd", lambda e: e.tensor_tensor(out=r2[:], in0=r2[:], in1=lnB[:], op=ALU.add), R=[r2, lnB], W=[r2])
    mk.dma("sync", dst_ap, r2[:], R=[r2])


def load_cast(g, dst, src, kk, ncols):
    mk = g.mk
    with mk.scope():
        stf = mk.pool("lcst", [128, 8, 512], F32, 2)
        srcr = src.rearrange("(k p) n -> p k n", p=128)
        for i, c0 in enumerate(range(0, ncols, 512)):
            w = min(512, ncols - c0)
            sf = stf.next()
            mk.dma("sync", sf[:, 0:kk, 0:w], srcr[:, :, c0:c0 + w], W=[sf])
            mk.do("gpsimd" if i % 2 else "vector", lambda e: e.tensor_copy(out=dst[:, :, c0:c0 + w], in_=sf[:, 0:kk, 0:w]), R=[sf], W=[dst], join=True)


def gather_weights(g):
    mk, nc = g.mk, g.nc
    g.wall = {}
    with mk.scope():
        stg = mk.pool("wstg", [128, 8, 512], BF16, 3)
        stf = mk.pool("wstf", [128, 8, 512], F32, 3)
        for name, src, kk, cols in (("wg", g.wg_sh, 8, 512), ("wu", g.wu_sh, 8, 512), ("wd", g.wd_sh, 4, D)):
            rows = 4 * kk * 128
            bounce = nc.dram_tensor(name + "_bnc", [rows, cols], BF16, kind="Internal").ap()
            full = nc.dram_tensor(name + "_all", [8 * rows, cols], BF16, kind="Internal").ap()
            tb, tf = T(name + "_bnc"), T(name + "_all")
            srcr = src.rearrange("(e k p) n -> e p k n", p=128, k=kk)
            bncr = bounce.rearrange("(e k p) n -> e p k n", p=128, k=kk)
            for e4 in range(4):
                st, sf = stg.next(), stf.next()
                view = st[:, :, :] if kk == 8 else st[:, :, :].rearrange("p (k a) n -> p k (a n)", a=2)
                viewf = sf[:, :, :] if kk == 8 else sf[:, :, :].rearrange("p (k a) n -> p k (a n)", a=2)
                mk.dma("sync", viewf, srcr[e4], W=[sf])
                mk.do("gpsimd" if e4 % 2 else "vector", lambda e: e.tensor_copy(out=st[:, :, :], in_=sf[:, :, :]), R=[sf], W=[st])
                mk.dma("sync", bncr[e4], view, R=[st], W=[tb], join=True)
            es = mk.E["gpsimd"]
            mk._deps(es, [tb], [tf], False, False)
            ins = nc.gpsimd.collective_compute("AllGather", ALU.bypass, replica_groups=[list(range(8))],
                                               ins=[bounce[:, :]], outs=[full[:, :]])
            tf.dsem = Sem(mk, "cc_" + name)
            mk.dsems.append(tf.dsem)
            sem, val = tf.dsem.bump(1)
            ins.then_inc(sem, 1)
            mk._record((sem, val, None), [tb], [tf], False)
            g.wall[name] = (full, tf)


def moe_phase(g, gB2):
    mk = g.mk
    with mk.scope():
        lnG = mk.sb("ln2g", [128, D], F32)
        lnB = mk.sb("ln2b", [128, D], F32)
        load_bcast(g, lnG, g.ln[2:3, :])
        load_bcast(g, lnB, g.ln[3:4, :])
        rwt = mk.sb("rwt", [128, 8, 36], F32)
        rbt = mk.sb("rbt", [128, 36], F32)
        mk.dma("sync", rwt[:], g.rw.rearrange("(k p) n -> p k n", p=128), W=[rwt])
        load_bcast(g, rbt, g.rb[0:1, :])
        H2 = mk.sb("H2", [128, 8, 1152], BF16)
        H2f = mk.pool("H2f", [128, 8, 128], F32, 2)
        wgp = mk.pool("wgb", [128, 8, 512], BF16, 2)
        wup = mk.pool("wub", [128, 8, 512], BF16, 2)
        wdp = mk.pool("wdb", [128, 4, D], BF16, 2)
        yacc = [mk.sb("yacc%d" % i, [128, D], F32) for i in range(9)]
        actp = mk.pool("act", [128, 4, 512], BF16, 2)
        sgp = mk.pool("sg", [128, 512], F32, 2)
        Gall = mk.sb("Gall", [128, 9, 32], F32)
        gt = mk.pool("gt", [128, 160], F32, 2)
        tp = {"r": mk.pool("lr", [128, D], F32, 1), "r2": mk.pool("lr2", [128, D], F32, 2), "st": mk.pool("lst", [128, 8], F32, 2)}
        wgr = g.wg.rearrange("(e k p) n -> e p k n", p=128, k=8)
        wur = g.wu.rearrange("(e k p) n -> e p k n", p=128, k=8)
        wdr = g.wd.rearrange("(e k p) n -> e p k n", p=128, k=4)
        all_tiles = [(0, j * 128) for j in range(NLAT // 128)] + [(1, 0), (1, 128)]
        sbs = [all_tiles[i:i + 9] for i in range(0, len(all_tiles), 9)]
        for sb_ in sbs:
            col = 0
            tiles = []
            for (s, tt0) in sb_:
                if True:
                    row = (tt0 if s == 0 else NLAT + tt0)
                    src = g.x1[row:row + 128, :]
                    hf = H2f.next()
                    load_hT(g, s, 0, 128, 3, 4, out_bf=None, out_f=hf, src=src)
                    ti = len(tiles)
                    tiles.append((s, tt0, col))
                    mk.do("gpsimd", lambda e: e.tensor_copy(out=H2[:, :, col:col + 128], in_=hf[:, :, :]), R=[hf], W=[H2], join=True)
                    pl = g.pY.next()
                    for k in range(8):
                        mk.do("tensor", lambda e: e.matmul(pl[:, 0:36], hf[:, k, :], rwt[:, k, :], start=(k == 0), stop=(k == 7)),
                              R=[hf, rwt], W=[pl])
                    t = gt.next()
                    lg = t[:, 0:36]
                    mk.do("vector", lambda e: e.tensor_tensor(out=lg, in0=pl[:, 0:36], in1=rbt[:], op=ALU.add), R=[pl, rbt], W=[t])
                    mk.do("vector", lambda e: e.reduce_max(out=t[:, 40:41], in_=t[:, 0:4], axis=AX.X), R=[t], W=[t])
                    mk.do("vector", lambda e: e.tensor_scalar(out=t[:, 41:42], in0=t[:, 40:41], scalar1=-1.0, scalar2=None, op0=ALU.mult), R=[t], W=[t])
                    mk.do("gpsimd", lambda e: e.memset(t[:, 42:43], 0.0), R=[t], W=[t])
                    mk.do("scalar", lambda e: e.activation(out=t[:, 44:48], in_=t[:, 0:4], func=AF.Exp, bias=t[:, 41:42], scale=1.0, accum_out=t[:, 42:43]),
                          R=[t], W=[t])
                    mk.do("vector", lambda e: e.reciprocal(out=t[:, 43:44], in_=t[:, 42:43]), R=[t], W=[t])
                    mk.do("vector", lambda e: e.tensor_scalar(out=t[:, 48:52], in0=t[:, 0:4], scalar1=t[:, 40:41], scalar2=None, op0=ALU.is_equal), R=[t], W=[t])
                    mk.do("vector", lambda e: e.tensor_scalar(out=t[:, 48:52], in0=t[:, 48:52], scalar1=-1.0, scalar2=1e30, op0=ALU.add, op1=ALU.mult), R=[t], W=[t])
                    for gi in range(4):
                        mk.do("vector", lambda e: e.tensor_scalar(out=t[:, 56 + gi * 8:64 + gi * 8], in0=t[:, 4 + gi * 8:12 + gi * 8],
                                                                   scalar1=t[:, 48 + gi:49 + gi], scalar2=None, op0=ALU.add), R=[t], W=[t])
                    m = t[:, 56:88]
                    mk.do("vector", lambda e: e.reduce_max(out=t[:, 52:53], in_=m, axis=AX.X), R=[t], W=[t])
                    mk.do("vector", lambda e: e.tensor_scalar(out=t[:, 88:120], in0=m, scalar1=t[:, 52:53], scalar2=None, op0=ALU.is_equal), R=[t], W=[t])
                    mk.do("vector", lambda e: e.scalar_tensor_tensor(out=t[:, 120:152], in0=t[:, 88:120], scalar=-1e30, in1=m, op0=ALU.mult, op1=ALU.add), R=[t], W=[t])
                    mk.do("vector", lambda e: e.reduce_max(out=t[:, 53:54], in_=t[:, 120:152], axis=AX.X), R=[t], W=[t])
                    mk.do("vector", lambda e: e.tensor_scalar(out=t[:, 120:152], in0=t[:, 120:152], scalar1=t[:, 53:54], scalar2=None, op0=ALU.is_equal), R=[t], W=[t])
                    mk.do("vector", lambda e: e.tensor_tensor(out=t[:, 54:55], in0=t[:, 53:54], in1=t[:, 52:53], op=ALU.subtract), R=[t], W=[t])
                    mk.do("scalar", lambda e: e.activation(out=t[:, 55:56], in_=t[:, 54:55], func=AF.Exp), R=[t], W=[t])
                    mk.do("vector", lambda e: e.tensor_scalar(out=t[:, 152:153], in0=t[:, 55:56], scalar1=1.0, scalar2=None, op0=ALU.add), R=[t], W=[t])
                    mk.do("vector", lambda e: e.reciprocal(out=t[:, 152:153], in_=t[:, 152:153]), R=[t], W=[t])
                    mk.do("vector", lambda e: e.tensor_tensor(out=t[:, 153:154], in0=t[:, 152:153], in1=t[:, 43:44], op=ALU.mult), R=[t], W=[t])
                    mk.do("vector", lambda e: e.tensor_tensor(out=t[:, 154:155], in0=t[:, 153:154], in1=t[:, 55:56], op=ALU.mult), R=[t], W=[t])
                    mk.do("vector", lambda e: e.tensor_scalar(out=t[:, 88:120], in0=t[:, 88:120], scalar1=t[:, 153:154], scalar2=None, op0=ALU.mult), R=[t], W=[t])
                    mk.do("vector", lambda e: e.scalar_tensor_tensor(out=Gall[:, ti, :], in0=t[:, 120:152], scalar=t[:, 154:155], in1=t[:, 88:120],
                                                                      op0=ALU.mult, op1=ALU.add), R=[t], W=[Gall], join=True)
                    col += 128
            ntok = col
            nblocks = [(b0, min(512, ntok - b0)) for b0 in range(0, ntok, 512)]
            for ex in range(NEXP):
                wgb, wub, wdb = wgp.next(), wup.next(), wdp.next()
                mk.dma("gpsimd", wgb[:], wgr[ex], W=[wgb])
                mk.dma("gpsimd", wub[:], wur[ex], W=[wub])
                mk.dma("gpsimd", wdb[:], wdr[ex], W=[wdb])
                for (b0, n) in nblocks:
                    act = actp.next()
                    for hc in range(4):
                        pg = g.pM.next()
                        for k in range(8):
                            mk.do("tensor", lambda e: e.matmul(pg[:, 0:n], wgb[:, k, hc * 128:(hc + 1) * 128], H2[:, k, b0:b0 + n], start=(k == 0), stop=(k == 7)),
                                  R=[wgb, H2], W=[pg])
                        pu = g.pM.next()
                        for k in range(8):
                            mk.do("tensor", lambda e: e.matmul(pu[:, 0:n], wub[:, k, hc * 128:(hc + 1) * 128], H2[:, k, b0:b0 + n], start=(k == 0), stop=(k == 7)),
                                  R=[wub, H2], W=[pu])
                        sg = sgp.next()
                        mk.do("scalar", lambda e: e.activation(out=sg[:, 0:n], in_=pg[:, 0:n], func=AF.Silu), R=[pg], W=[sg])
                        mk.do("vector", lambda e: e.tensor_tensor(out=act[:, hc, 0:n], in0=pu[:, 0:n], in1=sg[:, 0:n], op=ALU.mult), R=[pu, sg], W=[act], join=True)
                    for j in range(n // 128):
                        ti = (b0 // 128) + j
                        for half in range(2):
                            py = g.pY.next()
                            for hc in range(4):
                                mk.do("tensor", lambda e: e.matmul(py[:], act[:, hc, j * 128:(j + 1) * 128], wdb[:, hc, half * 512:(half + 1) * 512], start=(hc == 0), stop=(hc == 3)),
                                      R=[act, wdb], W=[py])
                            ya = yacc[ti]
                            if ex == 0:
                                mk.do("vector", lambda e: e.tensor_scalar(out=ya[:, half * 512:(half + 1) * 512], in0=py[:], scalar1=Gall[:, ti, ex:ex + 1], scalar2=None, op0=ALU.mult),
                                      R=[py, Gall], W=[ya], join=(half == 1))
                            else:
                                mk.do("vector", lambda e: e.scalar_tensor_tensor(out=ya[:, half * 512:(half + 1) * 512], in0=py[:], scalar=Gall[:, ti, ex:ex + 1],
                                                                                  in1=ya[:, half * 512:(half + 1) * 512], op0=ALU.mult, op1=ALU.add),
                                      R=[py, Gall, ya], W=[ya])
            for ti, (s, t0, c0) in enumerate(tiles):
                xt = g.xt.next()
                row = (t0 if s == 0 else NLAT + t0)
                mk.dma("sync", xt[:], g.x1[row:row + 128, :], W=[xt])
                dst = (g.ol if s == 0 else g.oc)[t0:t0 + 128, :]
                ya = yacc[ti]
                ln_epilogue(g, [ya[:, 0:512], ya[:, 512:1024]], [ya], xt, gB2[s], lnG, lnB, dst, tp)


def build_even():
    nc = bass.Bass("TRN2", target_bir_lowering=False)
    with ExitStack() as es:
        mk = MK(nc, es)
        g = setup_common(mk, nc, "even")
        din = g.din
        xh = din("xh", [30, D])
        hmask = din("hmask", [128, 30])
        w_in = din("w_in", [D, 2560])
        w_out = din("w_out", [D, D])
        dwa = din("dwa", [128, 4 * 31])
        dwb = din("dwb", [128, 4 * 3])
        cvec = din("cvec", [128, 12])
        NA_ = NLAT + 30
        NCX = NCTX + 30
        yad = nc.dram_tensor("yad", [4, 128, NA_], BF16, kind="Internal").ap()
        ud = nc.dram_tensor("ud", [4, 128, NA_], BF16, kind="Internal").ap()
        yacd = nc.dram_tensor("yacd", [4, 128, NCX], BF16, kind="Internal").ap()
        ucd = nc.dram_tensor("ucd", [4, 128, NCX], BF16, kind="Internal").ap()
        bgd = nc.dram_tensor("bgd", [4, 128, NTOK], BF16, kind="Internal").ap()

        gB = {(5, s): mk.sb("gB5%d" % s, [128, D], F32) for s in range(2)}
        es_g1 = ExitStack()
        old_es = mk.es
        mk.es = es_g1
        for s in range(2):
            gB[(2, s)] = mk.sb("gB2%d" % s, [128, D], F32)
        phase_ada(g, gB)
        dbg(g, "modc", g.modc, [128, 96])
        dbg(g, "g1lat", gB[(2, 0)], [128, D])
        dbg(g, "g2ctx", gB[(5, 1)], [128, D])

        with mk.scope():
            winb = mk.sb("winb", [128, 8, 2560], BF16)
            load_cast(g, winb, w_in, 8, 2560)
            hTp = mk.pool("hT", [128, 8, 512], BF16, 2)
            sgp = mk.pool("sgB", [128, 512], F32, 3)
            outp = mk.pool("outB", [128, 512], BF16, 6)
            hm = mk.sb("hm", [128, 30], F32)
            zt = mk.sb("zt", [128, 16], BF16)
            mk.dma("sync", hm[:], hmask[:, :], W=[hm])
            mk.do("vector", lambda e: e.memset(zt[:], 0.0), W=[zt])
            for c in range(4):
                for dd in (yacd, ucd):
                    mk.dma("sync", dd[c, :, 0:15], zt[:, 0:15], R=[zt])
                    mk.dma("sync", dd[c, :, 15 + NCTX:30 + NCTX], zt[:, 0:15], R=[zt])
            blocks = [("lat", 0, b * 512, 512) for b in range(NLAT // 512)] + [("halo", 0, 0, 30), ("ctx", 1, 0, 256)]
            for (bk, s, t0, n) in blocks:
                hT = hTp.next()
                load_hT(g, s, t0, n, 0, 1, out_bf=hT, src=(xh if bk == "halo" else None))

                def proj(cc):
                    p = g.pM.next()
                    for k in range(8):
                        mk.do("tensor", lambda e: e.matmul(p[:, 0:n], winb[:, k, cc * 128:(cc + 1) * 128], hT[:, k, 0:n], start=(k == 0), stop=(k == 7)),
                              R=[winb, hT], W=[p])
                    return p

                def store(o, dlat, dctx, c):
                    if bk == "lat":
                        mk.dma("sync", dlat[c, :, 15 + t0:15 + t0 + n], o[:, 0:n], R=[o])
                    elif bk == "ctx":
                        mk.dma("sync", dctx[c, :, 15:15 + n], o[:, 0:n], R=[o])
                    else:
                        mk.dma("sync", dlat[c, :, 0:15], o[:, 0:15], R=[o])
                        mk.dma("sync", dlat[c, :, 15 + NLAT:30 + NLAT], o[:, 15:30], R=[o])

                for c in range(4):
                    pgt = proj(4 + c)
                    sg = sgp.next()
                    mk.do("scalar", lambda e: e.activation(out=sg[:, 0:n], in_=pgt[:, 0:n], func=AF.Sigmoid), R=[pgt], W=[sg])
                    pv = proj(c)
                    o = outp.next()
                    if bk == "halo":
                        mk.do("vector", lambda e: e.tensor_tensor(out=sg[:, 0:n], in0=sg[:, 0:n], in1=hm[:, 0:n], op=ALU.mult), R=[sg, hm], W=[sg])
                    mk.do("vector", lambda e: e.tensor_tensor(out=o[:, 0:n], in0=pv[:, 0:n], in1=sg[:, 0:n], op=ALU.mult), R=[pv, sg], W=[o])
                    store(o, yad, yacd, c)
                    pc = proj(12 + c)
                    cg = sgp.next()
                    mk.do("scalar", lambda e: e.activation(out=cg[:, 0:n], in_=pc[:, 0:n], func=AF.Copy), R=[pc], W=[cg])
                    pb = proj(16 + c)
                    o2 = outp.next()
                    if bk == "halo":
                        mk.do("vector", lambda e: e.tensor_tensor(out=cg[:, 0:n], in0=cg[:, 0:n], in1=hm[:, 0:n], op=ALU.mult), R=[cg, hm], W=[cg])
                    mk.do("vector", lambda e: e.tensor_tensor(out=o2[:, 0:n], in0=pb[:, 0:n], in1=cg[:, 0:n], op=ALU.mult), R=[pb, cg], W=[o2])
                    store(o2, ud, ucd, c)
                    if bk != "halo":
                        pbg = proj(8 + c)
                        o3 = outp.next()
                        mk.do("scalar", lambda e: e.activation(out=o3[:, 0:n], in_=pbg[:, 0:n], func=AF.Copy), R=[pbg], W=[o3])
                        col0 = t0 if bk == "lat" else NLAT
                        mk.dma("sync", bgd[c, :, col0:col0 + n], o3[:, 0:n], R=[o3])
        mk.barrier()

        with mk.scope():
            woutb = mk.sb("woutb", [128, 8, D], BF16)
            load_cast(g, woutb, w_out, 8, D)
            dwat = mk.sb("dwat", [128, 4 * 31], F32)
            dwbt = mk.sb("dwbt", [128, 12], F32)
            cv = mk.sb("cv", [128, 12], F32)
            mk.dma("sync", dwat[:], dwa[:, :], W=[dwat])
            mk.dma("sync", dwbt[:], dwb[:, :], W=[dwbt])
            mk.dma("sync", cv[:], cvec[:, :], W=[cv])
            diagA = mk.sb("diagA", [128, 4 * 31, 128], BF16)
            diagB = mk.sb("diagB", [128, 12, 128], BF16)
            onesM = mk.sb("onesM", [128, 128], F32)
            mk.do("vector", lambda e: e.memset(onesM[:], 1.0 / 512.0), W=[onesM])
            for i in range(4 * 31):
                mk.do("vector" if i % 2 else "gpsimd", lambda e: e.tensor_scalar(out=diagA[:, i, :], in0=g.ident[:], scalar1=dwat[:, i:i + 1], scalar2=None, op0=ALU.mult),
                      R=[g.ident, dwat], W=[diagA], join=True)
            for i in range(12):
                mk.do("vector", lambda e: e.tensor_scalar(out=diagB[:, i, :], in0=g.ident[:], scalar1=dwbt[:, i:i + 1], scalar2=None, op0=ALU.mult),
                      R=[g.ident, dwbt], W=[diagB], join=True)
            lnG = mk.sb("ln1g", [128, D], F32)
            lnB = mk.sb("ln1b", [128, D], F32)
            load_bcast(g, lnG, g.ln[0:1, :])
            load_bcast(g, lnB, g.ln[1:2, :])
            yawp = mk.pool("yaw", [128, 4, 542], BF16, 2)
            uwp = mk.pool("uw", [128, 4, 542], BF16, 2)
            bgp = mk.pool("bgw", [128, 4, 512], BF16, 2)
            ya2 = mk.sb("ya2", [128, 4, 512], F32)
            sq = mk.sb("sq", [128, 4, 512], F32)
            msb = mk.sb("msb", [128, 512], F32)
            rsb = mk.sb("rsb", [128, 512], F32)
            tt = mk.pool("ttmp", [128, 512], F32, 2)
            catp = mk.pool("cat", [128, 8, 512], BF16, 2)
            tp = {"r": mk.pool("lr", [128, D], F32, 2), "r2": mk.pool("lr2", [128, D], F32, 2), "st": mk.pool("lst", [128, 8], F32, 2)}
            blocks = [(0, b * 512, 512) for b in range(NLAT // 512)] + [(1, 0, 256)]
            for (s, t0, n) in blocks:
                yaw, uw, bgw = yawp.next(), uwp.next(), bgp.next()
                srcA = yad if s == 0 else yacd
                srcU = ud if s == 0 else ucd
                mk.dma("sync", yaw[:, :, 0:n + 30], srcA[:, :, t0:t0 + n + 30].rearrange("c p t -> p c t"), W=[yaw])
                mk.dma("sync", uw[:, :, 0:n + 30], srcU[:, :, t0:t0 + n + 30].rearrange("c p t -> p c t"), W=[uw])
                col0 = t0 if s == 0 else NLAT
                mk.dma("sync", bgw[:, :, 0:n], bgd[:, :, col0:col0 + n].rearrange("c p t -> p c t"), W=[bgw])
                cat = catp.next()
                for c in range(4):
                    p = g.pM.next()
                    for k in range(31):
                        mk.do("tensor", lambda e: e.matmul(p[:, 0:n], diagA[:, c * 31 + k, :], yaw[:, c, k:k + n], start=(k == 0), stop=(k == 30)),
                              R=[diagA, yaw], W=[p])
                    mk.do("scalar", lambda e: e.activation(out=ya2[:, c, 0:n], in_=p[:, 0:n], func=AF.Identity, bias=cv[:, c:c + 1], scale=1.0),
                          R=[p, cv], W=[ya2], join=True)
                    mk.do("scalar", lambda e: e.activation(out=sq[:, c, 0:n], in_=p[:, 0:n], func=AF.Square, bias=cv[:, c:c + 1], scale=1.0),
                          R=[p, cv], W=[sq], join=True)
                pm = g.pM.next()
                for c in range(4):
                    mk.do("tensor", lambda e: e.matmul(pm[:, 0:n], onesM[:], ya2[:, c, 0:n], start=(c == 0), stop=(c == 3)), R=[onesM, ya2], W=[pm])
                pq = g.pM.next()
                for c in range(4):
                    mk.do("tensor", lambda e: e.matmul(pq[:, 0:n], onesM[:], sq[:, c, 0:n], start=(c == 0), stop=(c == 3)), R=[onesM, sq], W=[pq])
                mk.do("scalar", lambda e: e.activation(out=msb[:, 0:n], in_=pm[:, 0:n], func=AF.Copy), R=[pm], W=[msb])
                mk.do("vector", lambda e: e.tensor_tensor(out=rsb[:, 0:n], in0=msb[:, 0:n], in1=msb[:, 0:n], op=ALU.mult), R=[msb], W=[rsb])
                mk.do("vector", lambda e: e.tensor_tensor(out=rsb[:, 0:n], in0=pq[:, 0:n], in1=rsb[:, 0:n], op=ALU.subtract), R=[pq, rsb], W=[rsb])
                mk.do("scalar", lambda e: e.activation(out=rsb[:, 0:n], in_=rsb[:, 0:n], func=AF.Sqrt, bias=LN_EPS, scale=1.0), R=[rsb], W=[rsb])
                mk.do("vector", lambda e: e.reciprocal(out=rsb[:, 0:n], in_=rsb[:, 0:n]), R=[rsb], W=[rsb])
                for c in range(4):
                    t1 = tt.next()
                    mk.do("vector", lambda e: e.tensor_tensor(out=t1[:, 0:n], in0=ya2[:, c, 0:n], in1=msb[:, 0:n], op=ALU.subtract), R=[ya2, msb], W=[t1])
                    mk.do("gpsimd", lambda e: e.tensor_tensor(out=t1[:, 0:n], in0=t1[:, 0:n], in1=rsb[:, 0:n], op=ALU.mult), R=[t1, rsb], W=[t1])
                    mk.do("scalar", lambda e: e.activation(out=cat[:, c, 0:n], in_=t1[:, 0:n], func=AF.Silu, bias=cv[:, 8 + c:9 + c], scale=cv[:, 4 + c:5 + c]),
                          R=[t1, cv], W=[cat], join=True)
                for c in range(4):
                    p = g.pM.next()
                    for k in range(3):
                        mk.do("tensor", lambda e: e.matmul(p[:, 0:n], diagB[:, c * 3 + k, :], uw[:, c, 14 + k:14 + k + n], start=(k == 0), stop=(k == 2)),
                              R=[diagB, uw], W=[p])
                    mk.do("vector", lambda e: e.tensor_tensor(out=cat[:, 4 + c, 0:n], in0=p[:, 0:n], in1=bgw[:, c, 0:n], op=ALU.mult), R=[p, bgw], W=[cat], join=True)
                for j in range(n // 128):
                    pys = []
                    for half in range(2):
                        py = g.pY.next()
                        for kc in range(8):
                            mk.do("tensor", lambda e: e.matmul(py[:], cat[:, kc, j * 128:(j + 1) * 128], woutb[:, kc, half * 512:(half + 1) * 512], start=(kc == 0), stop=(kc == 7)),
                                  R=[cat, woutb], W=[py])
                        pys.append(py)
                    xt = g.xt.next()
                    mk.dma("sync", xt[:], tok_src(g, s, t0 + j * 128, 128), W=[xt])
                    row = (t0 if s == 0 else NLAT + t0) + j * 128
                    ln_epilogue(g, [pys[0][:], pys[1][:]], pys, xt, gB[(2, s)], lnG, lnB, g.x1[row:row + 128, :], tp)
        mk.barrier()
        mk.es = old_es
        es_g1.close()
        if not DEBUG.get("skip_moe"):
            moe_phase(g, {0: gB[(5, 0)], 1: gB[(5, 1)]})
        mk.barrier()
        g.nsem = mk.nsem
        g.ninst = {k: v.ninst for k, v in mk.E.items()}
        print("even: nsem", mk.nsem, g.ninst)
    return nc


_CACHE = {}


def _get(kind):
    if kind not in _CACHE:
        _CACHE[kind] = build_even() if kind == "even" else build_odd()
    return _CACHE[kind]


def _common_maps(i, xl, xc, c, c_ctx, ada_w, ada_b, lns, moe):
    maps = []
    ident = np.eye(128, dtype=np.float32)
    for core in range(NCORES):
        b, q = core // QPB, core % QPB
        cond = np.stack([c[b], c_ctx], axis=0)
        condT = np.ascontiguousarray(cond.reshape(2, 8, 128).transpose(2, 1, 0)).reshape(128, 16)
        m = {
            "xl": np.ascontiguousarray(xl[b, q * NLAT:(q + 1) * NLAT]),
            "xc": np.ascontiguousarray(xc[b]),
            "condT": condT,
            "ada_w": ada_w, "ada_b": ada_b.reshape(1, -1),
            "ln": lns, "ident": ident,
            "rw": moe["rw"], "rb": moe["rb"],
            "wg": moe["wg"].reshape(NEXP * D, 512), "wu": moe["wu"].reshape(NEXP * D, 512), "wd": moe["wd"].reshape(NEXP * 512, D),
        }
        maps.append(m)
    return maps


def make_maps(i, xl, xc, P):
    f = lambda a: np.ascontiguousarray(np.asarray(a, dtype=np.float32))
    c, c_ctx = f(P["c"]), f(P["c_ctx"])
    j = i // 2
    lns = np.stack([f(P["ln1_g"][i]), f(P["ln1_b"][i]), f(P["ln2_g"][i]), f(P["ln2_b"][i])], axis=0)
    moe = {"rw": np.ascontiguousarray(np.concatenate([f(P["router_w1"][i]), f(P["router_w2"][i])], axis=1)),
           "rb": np.concatenate([f(P["router_b1"][i]), f(P["router_b2"][i])]).reshape(1, 36),
           "wg": f(P["expert_w_gate"][i]), "wu": f(P["expert_w_up"][i]), "wd": f(P["expert_w_down"][i])}
    maps = _common_maps(i, xl, xc, c, c_ctx, f(P["ada_w"][i]), f(P["ada_b"][i]), lns, moe)
    if i % 2 == 0:
        dwa = np.ascontiguousarray(f(P["conv_a_dw_w"][j]).reshape(31, 4, 128).transpose(2, 1, 0)).reshape(128, 124)
        dwb = np.ascontiguousarray(f(P["conv_b_dw_w"][j]).reshape(3, 4, 128).transpose(2, 1, 0)).reshape(128, 12)
        cvec = np.concatenate([f(P["conv_a_dw_b"][j]).reshape(4, 128).T, f(P["conv_a_ln_g"][j]).reshape(4, 128).T,
                               f(P["conv_a_ln_b"][j]).reshape(4, 128).T], axis=1)
        w_in, w_out = f(P["conv_w_in"][j]), f(P["conv_w_out"][j])
        for core in range(NCORES):
            b, q = core // QPB, core % QPB
            xh = np.zeros((30, D), np.float32)
            hm = np.zeros((128, 30), np.float32)
            if q > 0:
                xh[0:15] = xl[b, q * NLAT - 15:q * NLAT]
                hm[:, 0:15] = 1.0
            if q < QPB - 1:
                xh[15:30] = xl[b, (q + 1) * NLAT:(q + 1) * NLAT + 15]
                hm[:, 15:30] = 1.0
            maps[core].update({"xh": xh, "hmask": hm, "w_in": w_in, "w_out": w_out,
                               "dwa": dwa, "dwb": dwb, "cvec": np.ascontiguousarray(cvec)})
        return "even", maps
    odd_maps(maps, xl, xc, f(P["attn_w_in"][j]), f(P["attn_w_out"][j]), f(P["q_norm_g"][j]), f(P["k_norm_g"][j]), f(P["na_rel_bias"][j]))
    return "odd", maps


def kernel(**P):
    n_layers = P.pop("n_layers", 4)
    f = lambda a: np.ascontiguousarray(np.asarray(a, dtype=np.float32))
    xl, xc = f(P["x"]), f(P["ctx"])
    for i in range(n_layers):
        kind, maps = make_maps(i, xl, xc, P)
        nc = _get(kind)
        res = run_bass_kernel_spmd(nc, maps, core_ids=list(range(NCORES)))
        xl = np.stack([np.concatenate([res.results[b * QPB + q]["ol"] for q in range(QPB)], axis=0) for b in range(2)], axis=0)
        xc = np.stack([res.results[b * QPB]["oc"] for b in range(2)], axis=0)
    return xl.astype(np.float32)


def build_odd():
    nc = bass.Bass("TRN2", target_bir_lowering=False)
    NT = NLAT + NCTX
    ROWS = NLAT // 64
    NQB = NLAT // 512
    with ExitStack() as es:
        mk = MK(nc, es)
        g = setup_common(mk, nc, "odd")
        din = g.din
        w_in = din("w_in", [D, 2304])
        w_out = din("w_out", [D, D])
        ropec = din("ropec", [128, NT])
        ropes = din("ropes", [128, NT])
        permd = din("permT", [128, 128])
        blkd = din("blk64", [128, 128])
        qkg = din("qkg", [128, 2])
        nab = din("nab", [3 * 8 * 12, 128, 512])

        def scr(name, shape):
            return nc.dram_tensor(name, list(shape), BF16, kind="Internal").ap()
        QC, KC = scr("QC", [4, 128, NT]), scr("KC", [2, 128, NT])
        QD, KD = scr("QD", [4, 128, NT]), scr("KD", [4, 128, NT])
        VC, VD = scr("VC", [NT, 2, 65]), scr("VD", [NT, 8, 65])
        OT = scr("OT", [8, 128, NT])

        gB = {(5, s): mk.sb("gB5%d" % s, [128, D], F32) for s in range(2)}
        es_g1 = ExitStack()
        old_es = mk.es
        mk.es = es_g1
        for s in range(2):
            gB[(2, s)] = mk.sb("gB2%d" % s, [128, D], F32)
        phase_ada(g, gB)
        blocks = [(0, b * 512, 512) for b in range(NQB)] + [(1, 0, 256)]

        def tokbase(s, t0):
            return t0 if s == 0 else NLAT + t0

        with mk.scope():
            winb = mk.sb("winb", [128, 8, 2304], BF16)
            load_cast(g, winb, w_in, 8, 2304)
            wkd = mk.sb("wkd", [128, 8, 2, 128], BF16)
            for h in range(2):
                for half in range(2):
                    mk.do("vector", lambda e: e.tensor_copy(out=wkd[:, :, h, half * 64:(half + 1) * 64], in_=winb[:, :, 512 + h * 64:512 + (h + 1) * 64]),
                          R=[winb], W=[wkd], join=True)
            permT = mk.sb("permT", [128, 128], F32)
            blk = mk.sb("blk", [128, 128], F32)
            gq = mk.sb("gq", [128, 2], F32)
            mk.dma("sync", permT[:], permd[:, :], W=[permT])
            mk.dma("sync", blk[:], blkd[:, :], W=[blk])
            mk.dma("sync", gq[:], qkg[:, :], W=[gq])
            hTp = mk.pool("hT", [128, 8, 512], BF16, 2)
            cosp = mk.pool("cos", [128, 512], F32, 2)
            sinp = mk.pool("sin", [128, 512], F32, 2)
            sqp = mk.pool("sq", [128, 512], F32, 2)
            rsp = mk.pool("rs", [128, 512], F32, 2)
            xnp = mk.pool("xn", [128, 512], F32, 2)
            t1p = mk.pool("t1", [128, 512], F32, 2)
            obp = mk.pool("ob", [128, 512], BF16, 4)
            vcp = mk.pool("vco", [128, 2, 65], BF16, 2)
            vdp = mk.pool("vdo", [128, 8, 65], BF16, 2)
            for (s, t0, n) in blocks:
                tb = tokbase(s, t0)
                hT = hTp.next()
                load_hT(g, s, t0, n, 0, 1, out_bf=hT)
                cs, sn = cosp.next(), sinp.next()
                mk.dma("sync", cs[:, 0:n], ropec[:, tb:tb + n], W=[cs])
                mk.dma("sync", sn[:, 0:n], ropes[:, tb:tb + n], W=[sn])

                def proj(lhs_fn):
                    p = g.pM.next()
                    for k in range(8):
                        mk.do("tensor", lambda e: e.matmul(p[:, 0:n], lhs_fn(k), hT[:, k, 0:n], start=(k == 0), stop=(k == 7)),
                              R=[winb, wkd, hT], W=[p])
                    return p

                def norm_rope(p, gcol, dst):
                    sq, rs, xn, t1 = sqp.next(), rsp.next(), xnp.next(), t1p.next()
                    mk.do("scalar", lambda e: e.activation(out=sq[:, 0:n], in_=p[:, 0:n], func=AF.Square), R=[p], W=[sq])
                    pm = g.pY.next()
                    mk.do("tensor", lambda e: e.matmul(pm[:, 0:n], blk[:], sq[:, 0:n], start=True, stop=True), R=[blk, sq], W=[pm])
                    mk.do("scalar", lambda e: e.activation(out=rs[:, 0:n], in_=pm[:, 0:n], func=AF.Sqrt, bias=RMS_EPS, scale=1.0), R=[pm], W=[rs])
                    mk.do("vector", lambda e: e.reciprocal(out=rs[:, 0:n], in_=rs[:, 0:n]), R=[rs], W=[rs])
                    mk.do("vector", lambda e: e.scalar_tensor_tensor(out=xn[:, 0:n], in0=p[:, 0:n], scalar=gq[:, gcol:gcol + 1], in1=rs[:, 0:n],
                                                                      op0=ALU.mult, op1=ALU.mult), R=[p, gq, rs], W=[xn])
                    pr = g.pY.next()
                    mk.do("tensor", lambda e: e.matmul(pr[:, 0:n], permT[:], xn[:, 0:n], start=True, stop=True), R=[permT, xn], W=[pr])
                    mk.do("gpsimd", lambda e: e.tensor_tensor(out=t1[:, 0:n], in0=xn[:, 0:n], in1=cs[:, 0:n], op=ALU.mult), R=[xn, cs], W=[t1])
                    mk.do("vector", lambda e: e.tensor_tensor(out=xn[:, 0:n], in0=pr[:, 0:n], in1=sn[:, 0:n], op=ALU.mult), R=[pr, sn], W=[xn])
                    ob = obp.next()
                    mk.do("gpsimd", lambda e: e.tensor_tensor(out=ob[:, 0:n], in0=t1[:, 0:n], in1=xn[:, 0:n], op=ALU.add), R=[t1, xn], W=[ob])
                    mk.dma("sync", dst[:, tb:tb + n], ob[:, 0:n], R=[ob])

                for ch in range(4):
                    p = proj(lambda k: winb[:, k, ch * 128:(ch + 1) * 128])
                    norm_rope(p, 0, QC[ch])
                for h in range(2):
                    p = proj(lambda k: wkd[:, k, h, :])
                    norm_rope(p, 1, KC[h])
                for ch in range(4):
                    for (c0, dst) in ((768, QD), (1280, KD)):
                        p = proj(lambda k: winb[:, k, c0 + ch * 128:c0 + (ch + 1) * 128])
                        ob = obp.next()
                        mk.do("scalar", lambda e: e.activation(out=ob[:, 0:n], in_=p[:, 0:n], func=AF.Copy), R=[p], W=[ob])
                        mk.dma("sync", dst[ch, :, tb:tb + n], ob[:, 0:n], R=[ob])
                for j in range(n // 128):
                    pv = g.pM.next()
                    for k in range(8):
                        mk.do("tensor", lambda e: e.matmul(pv[:, 0:128], hT[:, k, j * 128:(j + 1) * 128], winb[:, k, 640:768], start=(k == 0), stop=(k == 7)),
                              R=[winb, hT], W=[pv])
                    vo = vcp.next()
                    mk.do("vector", lambda e: e.memset(vo[:, :, 64:65], 1.0), W=[vo])
                    mk.do("vector", lambda e: e.tensor_copy(out=vo[:, :, 0:64], in_=pv[:, 0:128].rearrange("p (h d) -> p h d", h=2)), R=[pv], W=[vo], join=True)
                    mk.dma("sync", VC[tb + j * 128:tb + (j + 1) * 128, :, :], vo[:, :, :], R=[vo])
                    pv2 = g.pM.next()
                    for k in range(8):
                        mk.do("tensor", lambda e: e.matmul(pv2[:, 0:512], hT[:, k, j * 128:(j + 1) * 128], winb[:, k, 1792:2304], start=(k == 0), stop=(k == 7)),
                              R=[winb, hT], W=[pv2])
                    vo2 = vdp.next()
                    mk.do("gpsimd", lambda e: e.memset(vo2[:, :, 64:65], 1.0), W=[vo2])
                    mk.do("scalar", lambda e: e.activation(out=vo2[:, :, 0:64], in_=pv2[:, 0:512].rearrange("p (h d) -> p h d", h=8), func=AF.Copy), R=[pv2], W=[vo2], join=True)
                    mk.dma("sync", VD[tb + j * 128:tb + (j + 1) * 128, :, :], vo2[:, :, :], R=[vo2])
        mk.barrier()

        def finish_head(po, n, ph, dst_ap, pools):
            dtmp, bsb, ot = pools["dtmp"].next(), pools["bsb"].next(), pools["ot"].next()
            mk.do("vector", lambda e: e.reciprocal(out=dtmp[64:65, 0:n], in_=po[64:65, 0:n]), R=[po], W=[dtmp])
            pb = g.pT.next()
            mk.do("tensor", lambda e: e.matmul(pb[0:64, 0:n], g.ones[64:65, 0:64], dtmp[64:65, 0:n], start=True, stop=True), R=[g.ones, dtmp], W=[pb])
            mk.do("scalar", lambda e: e.activation(out=bsb[0:64, 0:n], in_=pb[0:64, 0:n], func=AF.Copy), R=[pb], W=[bsb])
            mk.do("vector", lambda e: e.tensor_tensor(out=ot[ph * 64:(ph + 1) * 64, 0:n], in0=po[0:64, 0:n], in1=bsb[0:64, 0:n], op=ALU.mult), R=[po, bsb], W=[ot])
            mk.dma("sync", dst_ap, ot[ph * 64:(ph + 1) * 64, 0:n], R=[ot])

        with mk.scope():
            bint = mk.sb("bint", [128, 12, 512], F32)
            bedge = mk.sb("bedge", [128, 12, 512], F32)
            qdp = mk.pool("qdt", [128, 512], BF16, 2)
            kdp = mk.pool("kdt", [128, 14 * 128], BF16, 2)
            vdp2 = mk.pool("vdt", [128, 14, 65], BF16, 2)
            tmpp = mk.pool("natmp", [128, 512], F32, 2)
            ptp = mk.pool("napt", [128, 512], BF16, 3)
            pools = {"dtmp": mk.pool("dtmp", [128, 512], F32, 2), "bsb": mk.pool("bsb", [128, 512], F32, 2), "ot": mk.pool("ot", [128, 512], BF16, 2)}
            nabr = nab.rearrange("(v h c) p q -> v h p c q", v=3, h=8)
            for h in range(8):
                ph, ch = h % 2, h // 2
                hs = slice(ph * 64, (ph + 1) * 64)
                mk.dma("sync", bint[:], nabr[0, h], W=[bint])
                for qb in range(NQB + 1):
                    isctx = qb == NQB
                    n = 256 if isctx else 512
                    t0 = NLAT if isctx else qb * 512
                    qt, kt, vt = qdp.next(), kdp.next(), vdp2.next()
                    mk.dma("sync", qt[hs, 0:n], QD[ch, hs, t0:t0 + n], W=[qt])
                    mk.dma("sync", kt[hs, 0:256], KD[ch, hs, NLAT:NLAT + 256], W=[kt])
                    mk.dma("sync", vt[:, 0:2, :], VD[NLAT:NLAT + 256, h, :].rearrange("(c p) d -> p c d", p=128), W=[vt], join=True)
                    chunks = [(0, None), (1, None)]
                    bias = bint
                    if not isctx:
                        r0 = qb * 8
                        cl = [c for c in range(12) if 0 <= (r0 - 8 + 2 * c) < ROWS]
                        c_lo, c_hi = cl[0], cl[-1] + 1
                        ts = (r0 - 8 + 2 * c_lo) * 64
                        mk.dma("sync", kt[hs, 256 + c_lo * 128:256 + c_hi * 128], KD[ch, hs, ts:ts + (c_hi - c_lo) * 128], W=[kt], join=True)
                        mk.dma("sync", vt[:, 2 + c_lo:2 + c_hi, :], VD[ts:ts + (c_hi - c_lo) * 128, h, :].rearrange("(c p) d -> p c d", p=128), W=[vt], join=True)
                        chunks += [(2 + c, c) for c in cl]
                        if qb == 0 or qb == NQB - 1:
                            mk.dma("sync", bedge[:], nabr[1 if qb == 0 else 2, h], W=[bedge])
                            bias = bedge
                    po = g.pY.next()
                    for ci, (idx, c) in enumerate(chunks):
                        ps = g.pM.next()
                        mk.do("tensor", lambda e: e.matmul(ps[:, 0:n], kt[hs, idx * 128:(idx + 1) * 128], qt[hs, 0:n], start=True, stop=True), R=[kt, qt], W=[ps])
                        pt = ptp.next()
                        if c is None:
                            mk.do("scalar", lambda e: e.activation(out=pt[:, 0:n], in_=ps[:, 0:n], func=AF.Exp, scale=0.125), R=[ps], W=[pt])
                        else:
                            tm = tmpp.next()
                            mk.do("vector", lambda e: e.scalar_tensor_tensor(out=tm[:, 0:n], in0=ps[:, 0:n], scalar=0.125, in1=bias[:, c, 0:n], op0=ALU.mult, op1=ALU.add),
                                  R=[ps, bias], W=[tm])
                            mk.do("scalar", lambda e: e.activation(out=pt[:, 0:n], in_=tm[:, 0:n], func=AF.Exp), R=[tm], W=[pt])
                        mk.do("tensor", lambda e: e.matmul(po[0:65, 0:n], vt[:, idx, 0:65], pt[:, 0:n], start=(ci == 0), stop=(ci == len(chunks) - 1)), R=[vt, pt], W=[po])
                    finish_head(po, n, ph, OT[4 + ch, hs, t0:t0 + n], pools)

        with mk.scope():
            NKC = NT // 128
            kcs = [mk.sb("kcs%d" % h, [128, NT], BF16) for h in range(2)]
            vcs = mk.sb("vcs", [128, NKC, 2, 65], BF16)
            for h in range(2):
                for i, c0 in enumerate(range(0, NT, 4096)):
                    c1 = min(NT, c0 + 4096)
                    mk.dma("sync", kcs[h][:, c0:c1], KC[h, :, c0:c1], W=[kcs[h]], join=True)
            for c0 in range(0, NKC, 26):
                c1 = min(NKC, c0 + 26)
                mk.dma("sync", vcs[:, c0:c1, :, :], VC[c0 * 128:c1 * 128, :, :].rearrange("(c p) h d -> p c h d", p=128), W=[vcs], join=True)
            qcp = mk.pool("qct", [128, 512], BF16, 2)
            ptp = mk.pool("gpt", [128, 512], BF16, 4)
            pools = {"dtmp": mk.pool("dtmp", [128, 512], F32, 2), "bsb": mk.pool("bsb", [128, 512], F32, 2), "ot": mk.pool("ot", [128, 512], BF16, 2)}
            for h in range(8):
                ph, ch, kvh = h % 2, h // 2, h // 4
                hs = slice(ph * 64, (ph + 1) * 64)
                for qb in range(NQB + 1):
                    isctx = qb == NQB
                    n = 256 if isctx else 512
                    t0 = NLAT if isctx else qb * 512
                    qt = qcp.next()
                    mk.dma("sync", qt[hs, 0:n], QC[ch, hs, t0:t0 + n], W=[qt])
                    kchunks = list(range(NLAT // 128, NKC)) if isctx else list(range(NKC))
                    po = g.pY.next()
                    for ci, kc in enumerate(kchunks):
                        ps = g.pM.next()
                        mk.do("tensor", lambda e: e.matmul(ps[:, 0:n], kcs[kvh][hs, kc * 128:(kc + 1) * 128], qt[hs, 0:n], start=True, stop=True), R=[kcs[kvh], qt], W=[ps])
                        pt = ptp.next()
                        mk.do("scalar", lambda e: e.activation(out=pt[:, 0:n], in_=ps[:, 0:n], func=AF.Exp, scale=0.125), R=[ps], W=[pt])
                        mk.do("tensor", lambda e: e.matmul(po[0:65, 0:n], vcs[:, kc, kvh, 0:65], pt[:, 0:n], start=(ci == 0), stop=(ci == len(kchunks) - 1)), R=[vcs, pt], W=[po])
                    finish_head(po, n, ph, OT[ch, hs, t0:t0 + n], pools)
        mk.barrier()

        with mk.scope():
            woutb = mk.sb("woutb", [128, 8, D], BF16)
            load_cast(g, woutb, w_out, 8, D)
            lnG = mk.sb("ln1g", [128, D], F32)
            lnB = mk.sb("ln1b", [128, D], F32)
            load_bcast(g, lnG, g.ln[0:1, :])
            load_bcast(g, lnB, g.ln[1:2, :])
            catp = mk.pool("cat", [128, 8, 128], BF16, 3)
            tp = {"r": mk.pool("lr", [128, D], F32, 2), "r2": mk.pool("lr2", [128, D], F32, 2), "st": mk.pool("lst", [128, 8], F32, 2)}
            for (s, t0, n) in blocks:
                for j in range(n // 128):
                    row = tokbase(s, t0) + j * 128
                    cat = catp.next()
                    mk.dma("sync", cat[:, :, :], OT[:, :, row:row + 128].rearrange("c p t -> p c t"), W=[cat])
                    pys = []
                    for half in range(2):
                        py = g.pY.next()
                        for kc in range(8):
                            mk.do("tensor", lambda e: e.matmul(py[:], cat[:, kc, :], woutb[:, kc, half * 512:(half + 1) * 512], start=(kc == 0), stop=(kc == 7)),
                                  R=[cat, woutb], W=[py])
                        pys.append(py)
                    xt = g.xt.next()
                    mk.dma("sync", xt[:], tok_src(g, s, t0 + j * 128, 128), W=[xt])
                    ln_epilogue(g, [pys[0][:], pys[1][:]], pys, xt, gB[(2, s)], lnG, lnB, g.x1[row:row + 128, :], tp)
        mk.barrier()
        mk.es = old_es
        es_g1.close()
        if not DEBUG.get("skip_moe"):
            moe_phase(g, {0: gB[(5, 0)], 1: gB[(5, 1)]})
        mk.barrier()
        print("odd: nsem", mk.nsem, {k: v.ninst for k, v in mk.E.items()})
    return nc


def odd_maps(maps, xl, xc, w_in, w_out, qg, kg, rel_bias):
    NT = NLAT + NCTX
    ROWS = NLAT // 64
    t = np.arange(NLAT)
    row, col = (t // 64).astype(np.float64), (t % 64).astype(np.float64)
    inv = 10000.0 ** (-np.arange(0, 32, 2, dtype=np.float64) / 32)
    cos = np.ones((128, NT), np.float32)
    sin = np.zeros((128, NT), np.float32)
    permT = np.zeros((128, 128), np.float32)
    for p in range(128):
        d = p % 64
        dd = d % 32
        pos = row if d < 32 else col
        ang = pos * inv[dd % 16]
        cos[p, :NLAT] = np.cos(ang)
        sin[p, :NLAT] = (-1.0 if dd < 16 else 1.0) * np.sin(ang)
        partner = p + 16 if dd < 16 else p - 16
        permT[partner, p] = 1.0
    blk = np.zeros((128, 128), np.float32)
    blk[0:64, 0:64] = 1.0 / 64
    blk[64:128, 64:128] = 1.0 / 64
    qkg = np.stack([np.tile(qg, 2), np.tile(kg, 2)], axis=1).astype(np.float32)
    nab = np.full((3, 8, 12, 2, 64, 8, 64), NEG, np.float32)
    kc = np.arange(64)[:, None]
    qc = np.arange(64)[None, :]
    cstart = np.clip(qc - 8, 0, 48)
    colok = (kc >= cstart) & (kc < cstart + 16)
    cidx = np.clip(kc - qc + 15, 0, 30)
    for v, r0 in enumerate((8, 0, ROWS - 8)):
        for c in range(12):
            for kr in range(2):
                krow = r0 - 8 + 2 * c + kr
                if not (0 <= krow < ROWS):
                    continue
                for qr in range(8):
                    qrow = r0 + qr
                    rs = min(max(qrow - 4, 0), ROWS - 8)
                    if not (rs <= krow < rs + 8):
                        continue
                    vals = rel_bias[:, krow - qrow + 7, :][:, cidx]
                    nab[v, :, c, kr, :, qr, :] = np.where(colok[None], vals, NEG)
    nab = np.ascontiguousarray(nab.reshape(3 * 8 * 12, 128, 512))
    for core in range(NCORES):
        maps[core].update({"w_in": w_in, "w_out": w_out, "ropec": cos, "ropes": sin, "permT": permT, "blk64": blk,
                           "qkg": qkg, "nab": nab})
```

```python
import numpy as np
from contextlib import ExitStack, contextmanager
import concourse.bass as bass
import concourse.mybir as mybir
from concourse.bass_utils import run_bass_kernel_spmd

F32 = mybir.dt.float32
BF16 = mybir.dt.bfloat16
AF = mybir.ActivationFunctionType
ALU = mybir.AluOpType
AX = mybir.AxisListType

SEM_LIMIT = 30000
D = 1024
NLAT = 16384
NCORES = 2
QPB = 1
NCTX = 256
NTOK = NLAT + NCTX
ALPHA = float((2 * 4) ** 0.25)
LN_EPS = 1e-5
RMS_EPS = 1e-6
NEXP = 32
NEG = -30000.0
DEBUG = {}
UID = [0]
DRAMS = {}


class Sem:
    def __init__(self, mk, name):
        self.mk, self.name, self.cur, self.val, self.n = mk, name, None, 0, 0

    def bump(self, inc):
        if self.cur is None or self.val + inc > SEM_LIMIT:
            self.mk.uid += 1
            self.cur = self.mk.nc.alloc_semaphore(name="%s_%d_%d" % (self.name, self.n, self.mk.uid))
            self.mk.sem_handles.append(self.cur)
            self.mk.nsem += 1
            self.n += 1
            self.val = 0
        self.val += inc
        return (self.cur, self.val)


class T:
    __slots__ = ("w", "r", "dsem", "name", "base")

    def __init__(self, name=""):
        self.w, self.r, self.dsem, self.name, self.base = [], [], None, name, []


class Tile:
    def __init__(self, t, tr):
        self.t, self.tr = t, tr

    def __getitem__(self, idx):
        return self.t[idx]


class EngState:
    def __init__(self, mk, name):
        self.name = name
        self.eng = getattr(mk.nc, name)
        self.sem = Sem(mk, "s_" + name)
        self.waited = {}
        self.ninst = 0


def _compact(lst):
    d = {}
    for t_ in lst:
        k = id(t_[0])
        if k not in d or d[k][1] < t_[1]:
            d[k] = t_
    return list(d.values())


class Pool:
    def __init__(self, tiles):
        self.tiles, self.i = tiles, 0

    def next(self):
        t = self.tiles[self.i % len(self.tiles)]
        self.i += 1
        return t


class MK:
    def __init__(self, nc, es):
        self.nc, self.es, self.es0 = nc, es, es
        self.nsem = 0
        self.E = {n: EngState(self, n) for n in ["tensor", "vector", "scalar", "gpsimd", "sync"]}
        self.dsems = []
        self.uid = UID[0]
        self.sem_handles = []

    def finish(self):
        self.barrier()
        self.nc.all_engine_barrier()
        self.nc.clear_and_free_semaphores(self.sem_handles)
        self.nc.all_engine_barrier()
        UID[0] = self.uid + 1

    @contextmanager
    def scope(self):
        old = self.es
        with ExitStack() as es2:
            self.es = es2
            try:
                yield
                self.barrier()
            finally:
                self.es = old

    def sb(self, name, shape, dt):
        self.uid += 1
        t = self.es.enter_context(self.nc.sbuf_tensor("%s_%d" % (name, self.uid), list(shape), dt))
        return Tile(t, T(name))

    def ps(self, name, shape, dt=F32):
        self.uid += 1
        t = self.es.enter_context(self.nc.psum_tensor("%s_%d" % (name, self.uid), list(shape), dt))
        return Tile(t, T(name))

    def pool(self, name, shape, dt, n, psum=False):
        return Pool([(self.ps if psum else self.sb)("%s%d" % (name, i), shape, dt) for i in range(n)])

    def _wait(self, es, tok):
        sem, val = tok[0], tok[1]
        key = id(sem)
        if es.waited.get(key, 0) >= val:
            return
        es.waited[key] = val
        es.eng.wait_ge(sem, val)

    def _deps(self, es, R, W, join, skip_same):
        for tr in R:
            for tok in tr.w:
                if not (skip_same and tok[2] is es):
                    self._wait(es, tok)
        for tr in W:
            for tok in (tr.base if join else tr.w):
                if not (skip_same and tok[2] is es):
                    self._wait(es, tok)
            for tok in tr.r:
                if not (skip_same and tok[2] is es):
                    self._wait(es, tok)

    def _record(self, tok3, R, W, join):
        for tr in R:
            tr.r.append(tok3)
            if len(tr.r) > 12:
                tr.r = _compact(tr.r)
        for tr in W:
            if join:
                tr.w.append(tok3)
                if len(tr.w) > 12:
                    tr.w = _compact(tr.w)
            else:
                tr.w = [tok3]
                tr.base = [tok3]
                tr.r = []

    @staticmethod
    def _trs(lst):
        return [x.tr if isinstance(x, Tile) else x for x in lst]

    def do(self, eng, fn, R=(), W=(), join=False):
        es = self.E[eng]
        R, W = self._trs(R), self._trs(W)
        self._deps(es, R, W, join, skip_same=(eng == "tensor"))
        ins = fn(es.eng)
        sem, val = es.sem.bump(1)
        ins.then_inc(sem, 1)
        es.ninst += 1
        self._record((sem, val, es), R, W, join)
        return ins

    def dma(self, q, out, in_, R=(), W=(), join=False, **kw):
        es = self.E[q]
        R, W = self._trs(R), self._trs(W)
        self._deps(es, R, W, join, skip_same=False)
        owner = (W + R)[0]
        if owner.dsem is None:
            owner.dsem = Sem(self, "d%d" % self.uid)
            self.uid += 1
            self.dsems.append(owner.dsem)
        ins = es.eng.dma_start(out=out, in_=in_, **kw)
        sem, val = owner.dsem.bump(16)
        ins.then_inc(sem, 16)
        es.ninst += 1
        self._record((sem, val, None), R, W, join)
        return ins

    def drain_dma(self, engs):
        for ds in self.dsems:
            if ds.cur is not None:
                for e in engs:
                    self._wait(self.E[e], (ds.cur, ds.val))

    def barrier(self, engs=("tensor", "vector", "scalar", "gpsimd", "sync")):
        for e in engs:
            es = self.E[e]
            for o in self.E.values():
                if o.sem.cur is not None and o is not es:
                    self._wait(es, (o.sem.cur, o.sem.val))
        self.drain_dma(engs)


class Ctx:
    pass


def dram(nc, name, shape, dt, kind):
    key = (id(nc), name)
    if key not in DRAMS:
        DRAMS[key] = nc.dram_tensor(name, list(shape), dt, kind=kind).ap()
    return DRAMS[key]


def setup_common(mk, nc, kind, sfx="", io=None):
    g = Ctx()
    g.mk, g.nc = mk, nc

    def din(name, shape, dt=F32, shared=False):
        return dram(nc, name + ("" if shared else sfx), shape, dt, "ExternalInput")

    g.din = din
    g.scr = lambda name, shape, dt=BF16: dram(nc, name, shape, dt, "Internal")
    if io is None:
        g.xl = din("xl", [NLAT, D])
        g.xc = din("xc", [NCTX, D])
        g.ol = dram(nc, "ol", [NLAT, D], F32, "ExternalOutput")
        g.oc = dram(nc, "oc", [NCTX, D], F32, "ExternalOutput")
    else:
        g.xl, g.xc, g.ol, g.oc = io
    g.condT = din("condT", [128, 16])
    g.ada_w = din("ada_w", [D, 6 * D])
    g.ada_b = din("ada_b", [1, 6 * D])
    g.ln = din("ln", [4, D])
    g.identd = din("ident", [128, 128], shared=True)
    g.rw = din("rw", [D, 36])
    g.rb = din("rb", [1, 36])
    g.wg = din("wg", [NEXP * D, 512])
    g.wu = din("wu", [NEXP * D, 512])
    g.wd = din("wd", [NEXP * 512, D])
    g.x1 = dram(nc, "x1s", [NTOK, D], F32, ("ExternalOutput" if DEBUG.get("x1out") else "Internal"))

    g.ident = mk.sb("ident", [128, 128], F32)
    g.ones = mk.sb("ones", [128, 128], F32)
    g.modc = mk.sb("modc", [128, 2 * 6 * 8], F32)
    mk.dma("sync", g.ident[:], g.identd[:, :], W=[g.ident])
    mk.do("vector", lambda e: e.memset(g.ones[:], 1.0), W=[g.ones])
    g.pT = mk.pool("pT", [128, 512], F32, 2, psum=True)
    g.pM = mk.pool("pM", [128, 512], F32, 4, psum=True)
    g.pY = mk.pool("pY", [128, 512], F32, 2, psum=True)
    g.xt = mk.pool("xt", [128, D], F32, 4)
    return g


def dbg(g, name, tile, shape, dt=F32):
    if not DEBUG.get("dump"):
        return
    o = g.nc.dram_tensor("dbg_" + name, list(shape), dt, kind="ExternalOutput").ap()
    g.mk.dma("sync", o, tile[:], R=[tile])


def tok_src(g, s, t0, n):
    return (g.xl if s == 0 else g.xc)[t0:t0 + n, :]


def mcol(g, s, v, c):
    i = (s * 6 + v) * 8 + c
    return g.modc[:, i:i + 1]


def phase_ada(g, gB):
    mk = g.mk
    with mk.scope():
        ct = mk.sb("condT", [128, 16], F32)
        sc = mk.sb("silu", [128, 16], F32)
        L = mk.sb("L", [128, 16, 128], F32)
        awp = mk.pool("aw", [128, 8, 512], F32, 2)
        abp = mk.pool("ab", [128, 512], F32, 2)
        tmp = mk.pool("adatmp", [128, 512], F32, 2)
        junk = mk.sb("adajunk", [128, 128], F32)
        mk.dma("sync", ct[:], g.condT[:, :], W=[ct])
        mk.do("scalar", lambda e: e.activation(out=sc[:], in_=ct[:], func=AF.Silu), R=[ct], W=[sc])
        for i in range(16):
            mk.do("vector", lambda e: e.tensor_scalar(out=L[:, i, :], in0=g.ones[:], scalar1=sc[:, i:i + 1],
                                                       scalar2=None, op0=ALU.mult), R=[g.ones, sc], W=[L], join=True)
        dbg(g, "sc", sc, [128, 16])
        dbg(g, "L", L, [128, 16, 128])
        awr = g.ada_w.rearrange("(k p) n -> p k n", p=128)
        for jb in range(12):
            aw = awp.next()
            ab = abp.next()
            mk.dma("sync", aw[:], awr[:, :, jb * 512:(jb + 1) * 512], W=[aw])
            mk.dma("sync", ab[:], g.ada_b[0:1, jb * 512:(jb + 1) * 512].partition_broadcast(128), W=[ab])
            v, half = jb // 2, jb % 2
            if jb == 4:
                dbg(g, "ab4", ab, [128, 512])
                dbg(g, "aw4", aw, [128, 8, 512])
            for s in range(2):
                p = g.pM.next()
                for k in range(8):
                    mk.do("tensor", lambda e: e.matmul(p[:], L[:, k * 2 + s, :], aw[:, k, :], start=(k == 0), stop=(k == 7)),
                          R=[L, aw], W=[p])
                if v in (2, 5):
                    dst = gB[(v, s)]
                    mk.do("vector", lambda e: e.tensor_tensor(out=dst[:, half * 512:(half + 1) * 512], in0=p[:], in1=ab[:], op=ALU.add),
                          R=[p, ab], W=[dst], join=True)
                else:
                    t = tmp.next()
                    mk.do("vector", lambda e: e.tensor_tensor(out=t[:], in0=p[:], in1=ab[:], op=ALU.add), R=[p, ab], W=[t])
                    for cc in range(4):
                        mk.do("vector", lambda e: e.tensor_tensor(out=junk[:], in0=t[:, cc * 128:(cc + 1) * 128], in1=g.ident[:], op=ALU.mult),
                              R=[t, g.ident], W=[junk])
                        i = (s * 6 + v) * 8 + half * 4 + cc
                        mk.do("vector", lambda e: e.reduce_sum(out=g.modc[:, i:i + 1], in_=junk[:], axis=AX.X),
                              R=[junk], W=[g.modc], join=True)
        for s in range(2):
            for v in (1, 4):
                i = (s * 6 + v) * 8
                mk.do("vector", lambda e: e.tensor_scalar_add(out=g.modc[:, i:i + 8], in0=g.modc[:, i:i + 8], scalar1=1.0),
                      R=[g.modc], W=[g.modc])


def load_bcast(g, dst, row_ap):
    g.mk.dma("sync", dst[:], row_ap.partition_broadcast(128), W=[dst])


def load_hT(g, s, t0, n, v_sh, v_sc, out_bf=None, out_f=None, src=None, f_eng="vector"):
    mk = g.mk
    nsub = (n + 127) // 128
    xts = []
    for j in range(nsub):
        r = min(128, n - j * 128)
        xt = g.xt.next()
        sap = src[j * 128:j * 128 + r, :] if src is not None else tok_src(g, s, t0 + j * 128, r)
        mk.dma("sync", xt[0:r, :], sap, W=[xt])
        xts.append((xt, r))
    for c in range(8):
        p = g.pT.next()
        for j, (xt, r) in enumerate(xts):
            mk.do("tensor", lambda e: e.transpose(out=p[:, j * 128:j * 128 + r], in_=xt[0:r, c * 128:(c + 1) * 128], identity=g.ident[0:r, 0:r]),
                  R=[xt, g.ident], W=[p], join=(j > 0))
        if out_bf is not None:
            mk.do("scalar", lambda e: e.activation(out=out_bf[:, c, 0:n], in_=p[:, 0:n], func=AF.Identity,
                                                   bias=mcol(g, s, v_sh, c), scale=mcol(g, s, v_sc, c)),
                  R=[p, g.modc], W=[out_bf], join=True)
        if out_f is not None:
            mk.do(f_eng, lambda e: e.tensor_scalar(out=out_f[:, c, 0:n], in0=p[:, 0:n], scalar1=mcol(g, s, v_sc, c),
                                                   scalar2=mcol(g, s, v_sh, c), op0=ALU.mult, op1=ALU.add),
                  R=[p, g.modc], W=[out_f], join=True)


def ln_epilogue(g, yh, yR, xt, gBt, lnG, lnB, dst_ap, tp):
    mk = g.mk
    r = tp["r"].next()
    for h in range(2):
        mk.do("vector", lambda e: e.tensor_tensor(out=r[:, h * 512:(h + 1) * 512], in0=yh[h], in1=gBt[:, h * 512:(h + 1) * 512], op=ALU.mult),
              R=yR + [gBt], W=[r], join=(h == 1))
    r2 = tp["r2"].next()
    mk.do("vector", lambda e: e.scalar_tensor_tensor(out=r2[:], in0=xt[:], scalar=ALPHA, in1=r[:], op0=ALU.mult, op1=ALU.add),
          R=[xt, r], W=[r2])
    st = tp["st"].next()
    mk.do("gpsimd", lambda e: e.memset(st[:], 0.0), W=[st])
    mk.do("scalar", lambda e: e.activation(out=r[:], in_=r2[:], func=AF.Identity, accum_out=st[:, 0:1]), R=[r2], W=[r, st])
    mk.do("scalar", lambda e: e.activation(out=r[:], in_=r2[:], func=AF.Square, accum_out=st[:, 1:2]), R=[r2], W=[r, st])
    mk.do("vector", lambda e: e.tensor_scalar(out=st[:, 2:4], in0=st[:, 0:2], scalar1=1.0 / D, scalar2=None, op0=ALU.mult), R=[st], W=[st])
    mk.do("vector", lambda e: e.tensor_tensor(out=st[:, 4:5], in0=st[:, 2:3], in1=st[:, 2:3], op=ALU.mult), R=[st], W=[st])
    mk.do("vector", lambda e: e.tensor_tensor(out=st[:, 3:4], in0=st[:, 3:4], in1=st[:, 4:5], op=ALU.subtract), R=[st], W=[st])
    mk.do("scalar", lambda e: e.activation(out=st[:, 3:4], in_=st[:, 3:4], func=AF.Sqrt, bias=LN_EPS, scale=1.0), R=[st], W=[st])
    mk.do("vector", lambda e: e.reciprocal(out=st[:, 3:4], in_=st[:, 3:4]), R=[st], W=[st])
    mk.do("vector", lambda e: e.tensor_scalar(out=r[:], in0=r2[:], scalar1=st[:, 2:3], scalar2=st[:, 3:4], op0=ALU.subtract, op1=ALU.mult),
          R=[r2, st], W=[r])
    mk.do("gpsimd", lambda e: e.tensor_tensor(out=r2[:], in0=r[:], in1=lnG[:], op=ALU.mult), R=[r, lnG], W=[r2])
    mk.do("gpsimd", lambda e: e.tensor_tensor(out=r2[:], in0=r2[:], in1=lnB[:], op=ALU.add), R=[r2, lnB], W=[r2])
    mk.dma("sync", dst_ap, r2[:], R=[r2])


def load_cast(g, dst, src, kk, ncols):
    mk = g.mk
    with mk.scope():
        stf = mk.pool("lcst", [128, 8, 512], F32, 2)
        srcr = src.rearrange("(k p) n -> p k n", p=128)
        for i, c0 in enumerate(range(0, ncols, 512)):
            w = min(512, ncols - c0)
            sf = stf.next()
            mk.dma("sync", sf[:, 0:kk, 0:w], srcr[:, :, c0:c0 + w], W=[sf])
            mk.do("gpsimd" if i % 2 else "vector", lambda e: e.tensor_copy(out=dst[:, :, c0:c0 + w], in_=sf[:, 0:kk, 0:w]), R=[sf], W=[dst], join=True)


def gather_weights(g):
    mk, nc = g.mk, g.nc
    g.wall = {}
    with mk.scope():
        stg = mk.pool("wstg", [128, 8, 512], BF16, 3)
        stf = mk.pool("wstf", [128, 8, 512], F32, 3)
        for name, src, kk, cols in (("wg", g.wg_sh, 8, 512), ("wu", g.wu_sh, 8, 512), ("wd", g.wd_sh, 4, D)):
            rows = 4 * kk * 128
            bounce = nc.dram_tensor(name + "_bnc", [rows, cols], BF16, kind="Internal").ap()
            full = nc.dram_tensor(name + "_all", [8 * rows, cols], BF16, kind="Internal").ap()
            tb, tf = T(name + "_bnc"), T(name + "_all")
            srcr = src.rearrange("(e k p) n -> e p k n", p=128, k=kk)
            bncr = bounce.rearrange("(e k p) n -> e p k n", p=128, k=kk)
            for e4 in range(4):
                st, sf = stg.next(), stf.next()
                view = st[:, :, :] if kk == 8 else st[:, :, :].rearrange("p (k a) n -> p k (a n)", a=2)
                viewf = sf[:, :, :] if kk == 8 else sf[:, :, :].rearrange("p (k a) n -> p k (a n)", a=2)
                mk.dma("sync", viewf, srcr[e4], W=[sf])
                mk.do("gpsimd" if e4 % 2 else "vector", lambda e: e.tensor_copy(out=st[:, :, :], in_=sf[:, :, :]), R=[sf], W=[st])
                mk.dma("sync", bncr[e4], view, R=[st], W=[tb], join=True)
            es = mk.E["gpsimd"]
            mk._deps(es, [tb], [tf], False, False)
            ins = nc.gpsimd.collective_compute("AllGather", ALU.bypass, replica_groups=[list(range(8))],
                                               ins=[bounce[:, :]], outs=[full[:, :]])
            tf.dsem = Sem(mk, "cc_" + name)
            mk.dsems.append(tf.dsem)
            sem, val = tf.dsem.bump(1)
            ins.then_inc(sem, 1)
            mk._record((sem, val, None), [tb], [tf], False)
            g.wall[name] = (full, tf)


def moe_phase(g, gB2):
    mk = g.mk
    with mk.scope():
        lnG = mk.sb("ln2g", [128, D], F32)
        lnB = mk.sb("ln2b", [128, D], F32)
        load_bcast(g, lnG, g.ln[2:3, :])
        load_bcast(g, lnB, g.ln[3:4, :])
        rwt = mk.sb("rwt", [128, 8, 36], F32)
        rbt = mk.sb("rbt", [128, 36], F32)
        mk.dma("sync", rwt[:], g.rw.rearrange("(k p) n -> p k n", p=128), W=[rwt])
        load_bcast(g, rbt, g.rb[0:1, :])
        H2 = mk.sb("H2", [128, 8, 1536], BF16)
        H2f = mk.pool("H2f", [128, 8, 128], F32, 2)
        wgp = mk.pool("wgb", [128, 8, 512], BF16, 2)
        wup = mk.pool("wub", [128, 8, 512], BF16, 2)
        wdp = mk.pool("wdb", [128, 4, D], BF16, 2)
        yacc = [mk.sb("yacc%d" % i, [128, D], F32) for i in range(12)]
        actp = mk.pool("act", [128, 4, 512], BF16, 2)
        sgp = mk.pool("sg", [128, 512], F32, 2)
        Gall = mk.sb("Gall", [128, 12, 32], F32)
        gt = mk.pool("gt", [128, 160], F32, 2)
        tp = {"r": mk.pool("lr", [128, D], F32, 1), "r2": mk.pool("lr2", [128, D], F32, 2), "st": mk.pool("lst", [128, 8], F32, 2)}
        wgr = g.wg.rearrange("(e k p) n -> e p k n", p=128, k=8)
        wur = g.wu.rearrange("(e k p) n -> e p k n", p=128, k=8)
        wdr = g.wd.rearrange("(e k p) n -> e p k n", p=128, k=4)
        all_tiles = [(0, j * 128) for j in range(NLAT // 128)] + [(1, 0), (1, 128)]
        sbs = [all_tiles[i:i + 12] for i in range(0, len(all_tiles), 12)]
        wgc = g.scr("wgc", [NEXP, 128, 8 * 512])
        wuc = g.scr("wuc", [NEXP, 128, 8 * 512])
        wdc = g.scr("wdc", [NEXP, 128, 4 * D])
        tcg, tcu, tcd = T("wgc"), T("wuc"), T("wdc")
        for sbi, sb_ in enumerate(sbs):
            col = 0
            tiles = []
            for (s, tt0) in sb_:
                if True:
                    row = (tt0 if s == 0 else NLAT + tt0)
                    src = g.x1[row:row + 128, :]
                    hf = H2f.next()
                    load_hT(g, s, 0, 128, 3, 4, out_bf=None, out_f=hf, src=src)
                    ti = len(tiles)
                    tiles.append((s, tt0, col))
                    mk.do("gpsimd", lambda e: e.tensor_copy(out=H2[:, :, col:col + 128], in_=hf[:, :, :]), R=[hf], W=[H2], join=True)
                    pl = g.pY.next()
                    for k in range(8):
                        mk.do("tensor", lambda e: e.matmul(pl[:, 0:36], hf[:, k, :], rwt[:, k, :], start=(k == 0), stop=(k == 7)),
                              R=[hf, rwt], W=[pl])
                    t = gt.next()
                    lg = t[:, 0:36]
                    mk.do("vector", lambda e: e.tensor_tensor(out=lg, in0=pl[:, 0:36], in1=rbt[:], op=ALU.add), R=[pl, rbt], W=[t])
                    mk.do("vector", lambda e: e.reduce_max(out=t[:, 40:41], in_=t[:, 0:4], axis=AX.X), R=[t], W=[t])
                    mk.do("vector", lambda e: e.tensor_scalar(out=t[:, 41:42], in0=t[:, 40:41], scalar1=-1.0, scalar2=None, op0=ALU.mult), R=[t], W=[t])
                    mk.do("gpsimd", lambda e: e.memset(t[:, 42:43], 0.0), R=[t], W=[t])
                    mk.do("scalar", lambda e: e.activation(out=t[:, 44:48], in_=t[:, 0:4], func=AF.Exp, bias=t[:, 41:42], scale=1.0, accum_out=t[:, 42:43]),
                          R=[t], W=[t])
                    mk.do("vector", lambda e: e.reciprocal(out=t[:, 43:44], in_=t[:, 42:43]), R=[t], W=[t])
                    mk.do("vector", lambda e: e.tensor_scalar(out=t[:, 48:52], in0=t[:, 0:4], scalar1=t[:, 40:41], scalar2=None, op0=ALU.is_equal), R=[t], W=[t])
                    mk.do("vector", lambda e: e.tensor_scalar(out=t[:, 48:52], in0=t[:, 48:52], scalar1=-1.0, scalar2=1e30, op0=ALU.add, op1=ALU.mult), R=[t], W=[t])
                    for gi in range(4):
                        mk.do("vector", lambda e: e.tensor_scalar(out=t[:, 56 + gi * 8:64 + gi * 8], in0=t[:, 4 + gi * 8:12 + gi * 8],
                                                                   scalar1=t[:, 48 + gi:49 + gi], scalar2=None, op0=ALU.add), R=[t], W=[t])
                    m = t[:, 56:88]
                    mk.do("vector", lambda e: e.reduce_max(out=t[:, 52:53], in_=m, axis=AX.X), R=[t], W=[t])
                    mk.do("vector", lambda e: e.tensor_scalar(out=t[:, 88:120], in0=m, scalar1=t[:, 52:53], scalar2=None, op0=ALU.is_equal), R=[t], W=[t])
                    mk.do("vector", lambda e: e.scalar_tensor_tensor(out=t[:, 120:152], in0=t[:, 88:120], scalar=-1e30, in1=m, op0=ALU.mult, op1=ALU.add), R=[t], W=[t])
                    mk.do("vector", lambda e: e.reduce_max(out=t[:, 53:54], in_=t[:, 120:152], axis=AX.X), R=[t], W=[t])
                    mk.do("vector", lambda e: e.tensor_scalar(out=t[:, 120:152], in0=t[:, 120:152], scalar1=t[:, 53:54], scalar2=None, op0=ALU.is_equal), R=[t], W=[t])
                    mk.do("vector", lambda e: e.tensor_tensor(out=t[:, 54:55], in0=t[:, 53:54], in1=t[:, 52:53], op=ALU.subtract), R=[t], W=[t])
                    mk.do("scalar", lambda e: e.activation(out=t[:, 55:56], in_=t[:, 54:55], func=AF.Exp), R=[t], W=[t])
                    mk.do("vector", lambda e: e.tensor_scalar(out=t[:, 152:153], in0=t[:, 55:56], scalar1=1.0, scalar2=None, op0=ALU.add), R=[t], W=[t])
                    mk.do("vector", lambda e: e.reciprocal(out=t[:, 152:153], in_=t[:, 152:153]), R=[t], W=[t])
                    mk.do("vector", lambda e: e.tensor_tensor(out=t[:, 153:154], in0=t[:, 152:153], in1=t[:, 43:44], op=ALU.mult), R=[t], W=[t])
                    mk.do("vector", lambda e: e.tensor_tensor(out=t[:, 154:155], in0=t[:, 153:154], in1=t[:, 55:56], op=ALU.mult), R=[t], W=[t])
                    mk.do("vector", lambda e: e.tensor_scalar(out=t[:, 88:120], in0=t[:, 88:120], scalar1=t[:, 153:154], scalar2=None, op0=ALU.mult), R=[t], W=[t])
                    mk.do("vector", lambda e: e.scalar_tensor_tensor(out=Gall[:, ti, :], in0=t[:, 120:152], scalar=t[:, 154:155], in1=t[:, 88:120],
                                                                      op0=ALU.mult, op1=ALU.add), R=[t], W=[Gall], join=True)
                    col += 128
            ntok = col
            nblocks = [(b0, min(512, ntok - b0)) for b0 in range(0, ntok, 512)]
            for ex in range(NEXP):
                wgb, wub, wdb = wgp.next(), wup.next(), wdp.next()
                if sbi == 0:
                    mk.dma("gpsimd", wgb[:], wgr[ex], W=[wgb])
                    mk.dma("gpsimd", wub[:], wur[ex], W=[wub])
                    mk.dma("gpsimd", wdb[:], wdr[ex], W=[wdb])
                    if len(sbs) > 1:
                        mk.dma("sync", wgc[ex], wgb[:].rearrange("p k n -> p (k n)"), W=[tcg], R=[wgb], join=True)
                        mk.dma("sync", wuc[ex], wub[:].rearrange("p k n -> p (k n)"), W=[tcu], R=[wub], join=True)
                        mk.dma("sync", wdc[ex], wdb[:].rearrange("p k n -> p (k n)"), W=[tcd], R=[wdb], join=True)
                else:
                    mk.dma("sync", wgb[:].rearrange("p k n -> p (k n)"), wgc[ex], W=[wgb], R=[tcg])
                    mk.dma("sync", wub[:].rearrange("p k n -> p (k n)"), wuc[ex], W=[wub], R=[tcu])
                    mk.dma("sync", wdb[:].rearrange("p k n -> p (k n)"), wdc[ex], W=[wdb], R=[tcd])
                for (b0, n) in nblocks:
                    act = actp.next()
                    for hc in range(4):
                        pg = g.pM.next()
                        for k in range(8):
                            mk.do("tensor", lambda e: e.matmul(pg[:, 0:n], wgb[:, k, hc * 128:(hc + 1) * 128], H2[:, k, b0:b0 + n], start=(k == 0), stop=(k == 7)),
                                  R=[wgb, H2], W=[pg])
                        pu = g.pM.next()
                        for k in range(8):
                            mk.do("tensor", lambda e: e.matmul(pu[:, 0:n], wub[:, k, hc * 128:(hc + 1) * 128], H2[:, k, b0:b0 + n], start=(k == 0), stop=(k == 7)),
                                  R=[wub, H2], W=[pu])
                        sg = sgp.next()
                        mk.do("scalar", lambda e: e.activation(out=sg[:, 0:n], in_=pg[:, 0:n], func=AF.Silu), R=[pg], W=[sg])
                        mk.do("vector", lambda e: e.tensor_tensor(out=act[:, hc, 0:n], in0=pu[:, 0:n], in1=sg[:, 0:n], op=ALU.mult), R=[pu, sg], W=[act], join=True)
                    for j in range(n // 128):
                        ti = (b0 // 128) + j
                        for half in range(2):
                            py = g.pY.next()
                            for hc in range(4):
                                mk.do("tensor", lambda e: e.matmul(py[:], act[:, hc, j * 128:(j + 1) * 128], wdb[:, hc, half * 512:(half + 1) * 512], start=(hc == 0), stop=(hc == 3)),
                                      R=[act, wdb], W=[py])
                            ya = yacc[ti]
                            if ex == 0:
                                mk.do("vector", lambda e: e.tensor_scalar(out=ya[:, half * 512:(half + 1) * 512], in0=py[:], scalar1=Gall[:, ti, ex:ex + 1], scalar2=None, op0=ALU.mult),
                                      R=[py, Gall], W=[ya], join=(half == 1))
                            else:
                                mk.do("vector", lambda e: e.scalar_tensor_tensor(out=ya[:, half * 512:(half + 1) * 512], in0=py[:], scalar=Gall[:, ti, ex:ex + 1],
                                                                                  in1=ya[:, half * 512:(half + 1) * 512], op0=ALU.mult, op1=ALU.add),
                                      R=[py, Gall, ya], W=[ya])
            for ti, (s, t0, c0) in enumerate(tiles):
                xt = g.xt.next()
                row = (t0 if s == 0 else NLAT + t0)
                mk.dma("sync", xt[:], g.x1[row:row + 128, :], W=[xt])
                dst = (g.ol if s == 0 else g.oc)[t0:t0 + 128, :]
                ya = yacc[ti]
                ln_epilogue(g, [ya[:, 0:512], ya[:, 512:1024]], [ya], xt, gB2[s], lnG, lnB, dst, tp)


def build_even(nc=None, sfx="", io=None):
    standalone = nc is None
    if standalone:
        nc = bass.Bass("TRN2", target_bir_lowering=False)
    with ExitStack() as es:
        mk = MK(nc, es)
        g = setup_common(mk, nc, "even", sfx, io)
        din = g.din
        xh = din("xh", [30, D])
        hmask = din("hmask", [128, 30])
        w_in = din("w_in", [D, 2560])
        w_out = din("w_out", [D, D])
        dwa = din("dwa", [128, 4 * 31])
        dwb = din("dwb", [128, 4 * 3])
        cvec = din("cvec", [128, 12])
        NA_ = NLAT + 30
        NCX = NCTX + 30
        yad = g.scr("yad", [4, 128, NA_])
        ud = g.scr("ud", [4, 128, NA_])
        yacd = g.scr("yacd", [4, 128, NCX])
        ucd = g.scr("ucd", [4, 128, NCX])
        bgd = g.scr("bgd", [4, 128, NTOK])

        gB = {(5, s): mk.sb("gB5%d" % s, [128, D], F32) for s in range(2)}
        es_g1 = ExitStack()
        old_es = mk.es
        mk.es = es_g1
        for s in range(2):
            gB[(2, s)] = mk.sb("gB2%d" % s, [128, D], F32)
        phase_ada(g, gB)
        dbg(g, "modc", g.modc, [128, 96])
        dbg(g, "g1lat", gB[(2, 0)], [128, D])
        dbg(g, "g2ctx", gB[(5, 1)], [128, D])

        with mk.scope():
            winb = mk.sb("winb", [128, 8, 2560], BF16)
            load_cast(g, winb, w_in, 8, 2560)
            hTp = mk.pool("hT", [128, 8, 512], BF16, 2)
            sgp = mk.pool("sgB", [128, 512], F32, 3)
            outp = mk.pool("outB", [128, 512], BF16, 6)
            hm = mk.sb("hm", [128, 30], F32)
            zt = mk.sb("zt", [128, 16], BF16)
            mk.dma("sync", hm[:], hmask[:, :], W=[hm])
            mk.do("vector", lambda e: e.memset(zt[:], 0.0), W=[zt])
            for c in range(4):
                for dd in (yacd, ucd):
                    mk.dma("sync", dd[c, :, 0:15], zt[:, 0:15], R=[zt])
                    mk.dma("sync", dd[c, :, 15 + NCTX:30 + NCTX], zt[:, 0:15], R=[zt])
            blocks = [("lat", 0, b * 512, 512) for b in range(NLAT // 512)] + [("halo", 0, 0, 30), ("ctx", 1, 0, 256)]
            for (bk, s, t0, n) in blocks:
                hT = hTp.next()
                load_hT(g, s, t0, n, 0, 1, out_bf=hT, src=(xh if bk == "halo" else None))

                def proj(cc):
                    p = g.pM.next()
                    for k in range(8):
                        mk.do("tensor", lambda e: e.matmul(p[:, 0:n], winb[:, k, cc * 128:(cc + 1) * 128], hT[:, k, 0:n], start=(k == 0), stop=(k == 7)),
                              R=[winb, hT], W=[p])
                    return p

                def store(o, dlat, dctx, c):
                    if bk == "lat":
                        mk.dma("sync", dlat[c, :, 15 + t0:15 + t0 + n], o[:, 0:n], R=[o])
                    elif bk == "ctx":
                        mk.dma("sync", dctx[c, :, 15:15 + n], o[:, 0:n], R=[o])
                    else:
                        mk.dma("sync", dlat[c, :, 0:15], o[:, 0:15], R=[o])
                        mk.dma("sync", dlat[c, :, 15 + NLAT:30 + NLAT], o[:, 15:30], R=[o])

                for c in range(4):
                    pgt = proj(4 + c)
                    sg = sgp.next()
                    mk.do("scalar", lambda e: e.activation(out=sg[:, 0:n], in_=pgt[:, 0:n], func=AF.Sigmoid), R=[pgt], W=[sg])
                    pv = proj(c)
                    o = outp.next()
                    if bk == "halo":
                        mk.do("vector", lambda e: e.tensor_tensor(out=sg[:, 0:n], in0=sg[:, 0:n], in1=hm[:, 0:n], op=ALU.mult), R=[sg, hm], W=[sg])
                    mk.do("vector", lambda e: e.tensor_tensor(out=o[:, 0:n], in0=pv[:, 0:n], in1=sg[:, 0:n], op=ALU.mult), R=[pv, sg], W=[o])
                    store(o, yad, yacd, c)
                    pc = proj(12 + c)
                    cg = sgp.next()
                    mk.do("scalar", lambda e: e.activation(out=cg[:, 0:n], in_=pc[:, 0:n], func=AF.Copy), R=[pc], W=[cg])
                    pb = proj(16 + c)
                    o2 = outp.next()
                    if bk == "halo":
                        mk.do("vector", lambda e: e.tensor_tensor(out=cg[:, 0:n], in0=cg[:, 0:n], in1=hm[:, 0:n], op=ALU.mult), R=[cg, hm], W=[cg])
                    mk.do("vector", lambda e: e.tensor_tensor(out=o2[:, 0:n], in0=pb[:, 0:n], in1=cg[:, 0:n], op=ALU.mult), R=[pb, cg], W=[o2])
                    store(o2, ud, ucd, c)
                    if bk != "halo":
                        pbg = proj(8 + c)
                        o3 = outp.next()
                        mk.do("scalar", lambda e: e.activation(out=o3[:, 0:n], in_=pbg[:, 0:n], func=AF.Copy), R=[pbg], W=[o3])
                        col0 = t0 if bk == "lat" else NLAT
                        mk.dma("sync", bgd[c, :, col0:col0 + n], o3[:, 0:n], R=[o3])
        mk.barrier()

        with mk.scope():
            woutb = mk.sb("woutb", [128, 8, D], BF16)
            load_cast(g, woutb, w_out, 8, D)
            dwat = mk.sb("dwat", [128, 4 * 31], F32)
            dwbt = mk.sb("dwbt", [128, 12], F32)
            cv = mk.sb("cv", [128, 12], F32)
            mk.dma("sync", dwat[:], dwa[:, :], W=[dwat])
            mk.dma("sync", dwbt[:], dwb[:, :], W=[dwbt])
            mk.dma("sync", cv[:], cvec[:, :], W=[cv])
            diagA = mk.sb("diagA", [128, 4 * 31, 128], BF16)
            diagB = mk.sb("diagB", [128, 12, 128], BF16)
            onesM = mk.sb("onesM", [128, 128], F32)
            mk.do("vector", lambda e: e.memset(onesM[:], 1.0 / 512.0), W=[onesM])
            for i in range(4 * 31):
                mk.do("vector" if i % 2 else "gpsimd", lambda e: e.tensor_scalar(out=diagA[:, i, :], in0=g.ident[:], scalar1=dwat[:, i:i + 1], scalar2=None, op0=ALU.mult),
                      R=[g.ident, dwat], W=[diagA], join=True)
            for i in range(12):
                mk.do("vector", lambda e: e.tensor_scalar(out=diagB[:, i, :], in0=g.ident[:], scalar1=dwbt[:, i:i + 1], scalar2=None, op0=ALU.mult),
                      R=[g.ident, dwbt], W=[diagB], join=True)
            lnG = mk.sb("ln1g", [128, D], F32)
            lnB = mk.sb("ln1b", [128, D], F32)
            load_bcast(g, lnG, g.ln[0:1, :])
            load_bcast(g, lnB, g.ln[1:2, :])
            yawp = mk.pool("yaw", [128, 4, 542], BF16, 2)
            uwp = mk.pool("uw", [128, 4, 542], BF16, 2)
            bgp = mk.pool("bgw", [128, 4, 512], BF16, 2)
            ya2 = mk.sb("ya2", [128, 4, 512], F32)
            sq = mk.sb("sq", [128, 4, 512], F32)
            msb = mk.sb("msb", [128, 512], F32)
            rsb = mk.sb("rsb", [128, 512], F32)
            tt = mk.pool("ttmp", [128, 512], F32, 2)
            catp = mk.pool("cat", [128, 8, 512], BF16, 2)
            tp = {"r": mk.pool("lr", [128, D], F32, 2), "r2": mk.pool("lr2", [128, D], F32, 2), "st": mk.pool("lst", [128, 8], F32, 2)}
            blocks = [(0, b * 512, 512) for b in range(NLAT // 512)] + [(1, 0, 256)]
            for (s, t0, n) in blocks:
                yaw, uw, bgw = yawp.next(), uwp.next(), bgp.next()
                srcA = yad if s == 0 else yacd
                srcU = ud if s == 0 else ucd
                mk.dma("sync", yaw[:, :, 0:n + 30], srcA[:, :, t0:t0 + n + 30].rearrange("c p t -> p c t"), W=[yaw])
                mk.dma("sync", uw[:, :, 0:n + 30], srcU[:, :, t0:t0 + n + 30].rearrange("c p t -> p c t"), W=[uw])
                col0 = t0 if s == 0 else NLAT
                mk.dma("sync", bgw[:, :, 0:n], bgd[:, :, col0:col0 + n].rearrange("c p t -> p c t"), W=[bgw])
                cat = catp.next()
                for c in range(4):
                    p = g.pM.next()
                    for k in range(31):
                        mk.do("tensor", lambda e: e.matmul(p[:, 0:n], diagA[:, c * 31 + k, :], yaw[:, c, k:k + n], start=(k == 0), stop=(k == 30)),
                              R=[diagA, yaw], W=[p])
                    mk.do("scalar", lambda e: e.activation(out=ya2[:, c, 0:n], in_=p[:, 0:n], func=AF.Identity, bias=cv[:, c:c + 1], scale=1.0),
                          R=[p, cv], W=[ya2], join=True)
                    mk.do("scalar", lambda e: e.activation(out=sq[:, c, 0:n], in_=p[:, 0:n], func=AF.Square, bias=cv[:, c:c + 1], scale=1.0),
                          R=[p, cv], W=[sq], join=True)
                pm = g.pM.next()
                for c in range(4):
                    mk.do("tensor", lambda e: e.matmul(pm[:, 0:n], onesM[:], ya2[:, c, 0:n], start=(c == 0), stop=(c == 3)), R=[onesM, ya2], W=[pm])
                pq = g.pM.next()
                for c in range(4):
                    mk.do("tensor", lambda e: e.matmul(pq[:, 0:n], onesM[:], sq[:, c, 0:n], start=(c == 0), stop=(c == 3)), R=[onesM, sq], W=[pq])
                mk.do("scalar", lambda e: e.activation(out=msb[:, 0:n], in_=pm[:, 0:n], func=AF.Copy), R=[pm], W=[msb])
                mk.do("vector", lambda e: e.tensor_tensor(out=rsb[:, 0:n], in0=msb[:, 0:n], in1=msb[:, 0:n], op=ALU.mult), R=[msb], W=[rsb])
                mk.do("vector", lambda e: e.tensor_tensor(out=rsb[:, 0:n], in0=pq[:, 0:n], in1=rsb[:, 0:n], op=ALU.subtract), R=[pq, rsb], W=[rsb])
                mk.do("scalar", lambda e: e.activation(out=rsb[:, 0:n], in_=rsb[:, 0:n], func=AF.Sqrt, bias=LN_EPS, scale=1.0), R=[rsb], W=[rsb])
                mk.do("vector", lambda e: e.reciprocal(out=rsb[:, 0:n], in_=rsb[:, 0:n]), R=[rsb], W=[rsb])
                for c in range(4):
                    t1 = tt.next()
                    mk.do("vector", lambda e: e.tensor_tensor(out=t1[:, 0:n], in0=ya2[:, c, 0:n], in1=msb[:, 0:n], op=ALU.subtract), R=[ya2, msb], W=[t1])
                    mk.do("gpsimd", lambda e: e.tensor_tensor(out=t1[:, 0:n], in0=t1[:, 0:n], in1=rsb[:, 0:n], op=ALU.mult), R=[t1, rsb], W=[t1])
                    mk.do("scalar", lambda e: e.activation(out=cat[:, c, 0:n], in_=t1[:, 0:n], func=AF.Silu, bias=cv[:, 8 + c:9 + c], scale=cv[:, 4 + c:5 + c]),
                          R=[t1, cv], W=[cat], join=True)
                for c in range(4):
                    p = g.pM.next()
                    for k in range(3):
                        mk.do("tensor", lambda e: e.matmul(p[:, 0:n], diagB[:, c * 3 + k, :], uw[:, c, 14 + k:14 + k + n], start=(k == 0), stop=(k == 2)),
                              R=[diagB, uw], W=[p])
                    mk.do("vector", lambda e: e.tensor_tensor(out=cat[:, 4 + c, 0:n], in0=p[:, 0:n], in1=bgw[:, c, 0:n], op=ALU.mult), R=[p, bgw], W=[cat], join=True)
                for j in range(n // 128):
                    pys = []
                    for half in range(2):
                        py = g.pY.next()
                        for kc in range(8):
                            mk.do("tensor", lambda e: e.matmul(py[:], cat[:, kc, j * 128:(j + 1) * 128], woutb[:, kc, half * 512:(half + 1) * 512], start=(kc == 0), stop=(kc == 7)),
                                  R=[cat, woutb], W=[py])
                        pys.append(py)
                    xt = g.xt.next()
                    mk.dma("sync", xt[:], tok_src(g, s, t0 + j * 128, 128), W=[xt])
                    row = (t0 if s == 0 else NLAT + t0) + j * 128
                    ln_epilogue(g, [pys[0][:], pys[1][:]], pys, xt, gB[(2, s)], lnG, lnB, g.x1[row:row + 128, :], tp)
        mk.barrier()
        mk.es = old_es
        es_g1.close()
        if not DEBUG.get("skip_moe"):
            moe_phase(g, {0: gB[(5, 0)], 1: gB[(5, 1)]})
        mk.barrier()
        print("even: nsem", mk.nsem, {k: v.ninst for k, v in mk.E.items()})
        if not standalone:
            mk.finish()
    return nc


_CACHE = {}


def _get(kind):
    if kind not in _CACHE:
        _CACHE[kind] = build_even() if kind == "even" else build_odd()
    return _CACHE[kind]


def _common_maps(i, xl, xc, c, c_ctx, ada_w, ada_b, lns, moe):
    maps = []
    ident = np.eye(128, dtype=np.float32)
    for core in range(NCORES):
        b, q = core // QPB, core % QPB
        cond = np.stack([c[b], c_ctx], axis=0)
        condT = np.ascontiguousarray(cond.reshape(2, 8, 128).transpose(2, 1, 0)).reshape(128, 16)
        m = {
            "xl": np.ascontiguousarray(xl[b, q * NLAT:(q + 1) * NLAT]),
            "xc": np.ascontiguousarray(xc[b]),
            "condT": condT,
            "ada_w": ada_w, "ada_b": ada_b.reshape(1, -1),
            "ln": lns, "ident": ident,
            "rw": moe["rw"], "rb": moe["rb"],
            "wg": moe["wg"].reshape(NEXP * D, 512), "wu": moe["wu"].reshape(NEXP * D, 512), "wd": moe["wd"].reshape(NEXP * 512, D),
        }
        maps.append(m)
    return maps


def make_maps(i, xl, xc, P):
    f = lambda a: np.ascontiguousarray(np.asarray(a, dtype=np.float32))
    c, c_ctx = f(P["c"]), f(P["c_ctx"])
    j = i // 2
    lns = np.stack([f(P["ln1_g"][i]), f(P["ln1_b"][i]), f(P["ln2_g"][i]), f(P["ln2_b"][i])], axis=0)
    moe = {"rw": np.ascontiguousarray(np.concatenate([f(P["router_w1"][i]), f(P["router_w2"][i])], axis=1)),
           "rb": np.concatenate([f(P["router_b1"][i]), f(P["router_b2"][i])]).reshape(1, 36),
           "wg": f(P["expert_w_gate"][i]), "wu": f(P["expert_w_up"][i]), "wd": f(P["expert_w_down"][i])}
    maps = _common_maps(i, xl, xc, c, c_ctx, f(P["ada_w"][i]), f(P["ada_b"][i]), lns, moe)
    if i % 2 == 0:
        dwa = np.ascontiguousarray(f(P["conv_a_dw_w"][j]).reshape(31, 4, 128).transpose(2, 1, 0)).reshape(128, 124)
        dwb = np.ascontiguousarray(f(P["conv_b_dw_w"][j]).reshape(3, 4, 128).transpose(2, 1, 0)).reshape(128, 12)
        cvec = np.concatenate([f(P["conv_a_dw_b"][j]).reshape(4, 128).T, f(P["conv_a_ln_g"][j]).reshape(4, 128).T,
                               f(P["conv_a_ln_b"][j]).reshape(4, 128).T], axis=1)
        w_in, w_out = f(P["conv_w_in"][j]), f(P["conv_w_out"][j])
        for core in range(NCORES):
            b, q = core // QPB, core % QPB
            xh = np.zeros((30, D), np.float32)
            hm = np.zeros((128, 30), np.float32)
            if q > 0:
                xh[0:15] = xl[b, q * NLAT - 15:q * NLAT]
                hm[:, 0:15] = 1.0
            if q < QPB - 1:
                xh[15:30] = xl[b, (q + 1) * NLAT:(q + 1) * NLAT + 15]
                hm[:, 15:30] = 1.0
            maps[core].update({"xh": xh, "hmask": hm, "w_in": w_in, "w_out": w_out,
                               "dwa": dwa, "dwb": dwb, "cvec": np.ascontiguousarray(cvec)})
        return "even", maps
    odd_maps(maps, xl, xc, f(P["attn_w_in"][j]), f(P["attn_w_out"][j]), f(P["q_norm_g"][j]), f(P["k_norm_g"][j]), f(P["na_rel_bias"][j]))
    return "odd", maps


def build_fused(n_layers=4):
    nc = bass.Bass("TRN2", target_bir_lowering=False)
    x_in = dram(nc, "x_in", [NLAT, D], F32, "ExternalInput")
    c_in = dram(nc, "c_in", [NCTX, D], F32, "ExternalInput")
    out = dram(nc, "out", [NLAT, D], F32, "ExternalOutput")
    bufs = [(dram(nc, "xbuf%d" % i, [NLAT, D], F32, "Internal"), dram(nc, "cbuf%d" % i, [NCTX, D], F32, "Internal")) for i in range(2)]
    src = (x_in, c_in)
    for li in range(n_layers):
        last = li == n_layers - 1
        dst = (out, bufs[li % 2][1]) if last else bufs[li % 2]
        io = (src[0], src[1], dst[0], dst[1])
        (build_even if li % 2 == 0 else build_odd)(nc, "_L%d" % li, io)
        src = dst
    return nc


def kernel_fused(P, n_layers=4):
    f = lambda a: np.ascontiguousarray(np.asarray(a, dtype=np.float32))
    xl, xc = f(P["x"]), f(P["ctx"])
    shared = ("ident", "ropec", "ropes", "permT", "blk64", "xl", "xc")
    maps = [{"x_in": xl[b], "c_in": xc[b]} for b in range(NCORES)]
    for i in range(n_layers):
        kind, lm = make_maps(i, xl, xc, P)
        for core in range(NCORES):
            for k, v in lm[core].items():
                if k in ("xl", "xc"):
                    continue
                maps[core][k if k in shared else k + "_L%d" % i] = v
    if "fused" not in _CACHE:
        _CACHE["fused"] = build_fused(n_layers)
    res = run_bass_kernel_spmd(_CACHE["fused"], maps, core_ids=list(range(NCORES)))
    return np.stack([res.results[b]["out"] for b in range(2)], axis=0).astype(np.float32)


def kernel(**P):
    n_layers = P.pop("n_layers", 4)
    if not DEBUG.get("unfused"):
        return kernel_fused(P, n_layers)
    f = lambda a: np.ascontiguousarray(np.asarray(a, dtype=np.float32))
    xl, xc = f(P["x"]), f(P["ctx"])
    for i in range(n_layers):
        kind, maps = make_maps(i, xl, xc, P)
        nc = _get(kind)
        res = run_bass_kernel_spmd(nc, maps, core_ids=list(range(NCORES)))
        xl = np.stack([np.concatenate([res.results[b * QPB + q]["ol"] for q in range(QPB)], axis=0) for b in range(2)], axis=0)
        xc = np.stack([res.results[b * QPB]["oc"] for b in range(2)], axis=0)
    return xl.astype(np.float32)


def build_odd(nc=None, sfx="", io=None):
    standalone = nc is None
    if standalone:
        nc = bass.Bass("TRN2", target_bir_lowering=False)
    NT = NLAT + NCTX
    ROWS = NLAT // 64
    NQB = NLAT // 512
    with ExitStack() as es:
        mk = MK(nc, es)
        g = setup_common(mk, nc, "odd", sfx, io)
        din = g.din
        w_in = din("w_in", [D, 2304])
        w_out = din("w_out", [D, D])
        ropec = din("ropec", [128, NT], shared=True)
        ropes = din("ropes", [128, NT], shared=True)
        permd = din("permT", [128, 128], shared=True)
        blkd = din("blk64", [128, 128], shared=True)
        qkg = din("qkg", [128, 2])
        nab = din("nab", [3 * 8 * 12, 128, 512])

        scr = g.scr
        QC, KC = scr("QC", [4, 128, NT]), scr("KC", [2, 128, NT])
        QD, KD = scr("QD", [4, 128, NT]), scr("KD", [4, 128, NT])
        VC, VD = scr("VC", [NT, 2, 65]), scr("VD", [NT, 8, 65])
        OT = scr("OT", [8, 128, NT])

        gB = {(5, s): mk.sb("gB5%d" % s, [128, D], F32) for s in range(2)}
        es_g1 = ExitStack()
        old_es = mk.es
        mk.es = es_g1
        for s in range(2):
            gB[(2, s)] = mk.sb("gB2%d" % s, [128, D], F32)
        phase_ada(g, gB)
        blocks = [(0, b * 512, 512) for b in range(NQB)] + [(1, 0, 256)]

        def tokbase(s, t0):
            return t0 if s == 0 else NLAT + t0

        with mk.scope():
            winb = mk.sb("winb", [128, 8, 2304], BF16)
            load_cast(g, winb, w_in, 8, 2304)
            wkd = mk.sb("wkd", [128, 8, 2, 128], BF16)
            for h in range(2):
                for half in range(2):
                    mk.do("vector", lambda e: e.tensor_copy(out=wkd[:, :, h, half * 64:(half + 1) * 64], in_=winb[:, :, 512 + h * 64:512 + (h + 1) * 64]),
                          R=[winb], W=[wkd], join=True)
            permT = mk.sb("permT", [128, 128], F32)
            blk = mk.sb("blk", [128, 128], F32)
            gq = mk.sb("gq", [128, 2], F32)
            mk.dma("sync", permT[:], permd[:, :], W=[permT])
            mk.dma("sync", blk[:], blkd[:, :], W=[blk])
            mk.dma("sync", gq[:], qkg[:, :], W=[gq])
            hTp = mk.pool("hT", [128, 8, 512], BF16, 2)
            cosp = mk.pool("cos", [128, 512], F32, 2)
            sinp = mk.pool("sin", [128, 512], F32, 2)
            sqp = mk.pool("sq", [128, 512], F32, 2)
            rsp = mk.pool("rs", [128, 512], F32, 2)
            xnp = mk.pool("xn", [128, 512], F32, 2)
            t1p = mk.pool("t1", [128, 512], F32, 2)
            obp = mk.pool("ob", [128, 512], BF16, 4)
            vcp = mk.pool("vco", [128, 2, 65], BF16, 2)
            vdp = mk.pool("vdo", [128, 8, 65], BF16, 2)
            for (s, t0, n) in blocks:
                tb = tokbase(s, t0)
                hT = hTp.next()
                load_hT(g, s, t0, n, 0, 1, out_bf=hT)
                cs, sn = cosp.next(), sinp.next()
                mk.dma("sync", cs[:, 0:n], ropec[:, tb:tb + n], W=[cs])
                mk.dma("sync", sn[:, 0:n], ropes[:, tb:tb + n], W=[sn])

                def proj(lhs_fn):
                    p = g.pM.next()
                    for k in range(8):
                        mk.do("tensor", lambda e: e.matmul(p[:, 0:n], lhs_fn(k), hT[:, k, 0:n], start=(k == 0), stop=(k == 7)),
                              R=[winb, wkd, hT], W=[p])
                    return p

                def norm_rope(p, gcol, dst):
                    sq, rs, xn, t1 = sqp.next(), rsp.next(), xnp.next(), t1p.next()
                    mk.do("scalar", lambda e: e.activation(out=sq[:, 0:n], in_=p[:, 0:n], func=AF.Square), R=[p], W=[sq])
                    pm = g.pY.next()
                    mk.do("tensor", lambda e: e.matmul(pm[:, 0:n], blk[:], sq[:, 0:n], start=True, stop=True), R=[blk, sq], W=[pm])
                    mk.do("scalar", lambda e: e.activation(out=rs[:, 0:n], in_=pm[:, 0:n], func=AF.Sqrt, bias=RMS_EPS, scale=1.0), R=[pm], W=[rs])
                    mk.do("vector", lambda e: e.reciprocal(out=rs[:, 0:n], in_=rs[:, 0:n]), R=[rs], W=[rs])
                    mk.do("vector", lambda e: e.scalar_tensor_tensor(out=xn[:, 0:n], in0=p[:, 0:n], scalar=gq[:, gcol:gcol + 1], in1=rs[:, 0:n],
                                                                      op0=ALU.mult, op1=ALU.mult), R=[p, gq, rs], W=[xn])
                    pr = g.pY.next()
                    mk.do("tensor", lambda e: e.matmul(pr[:, 0:n], permT[:], xn[:, 0:n], start=True, stop=True), R=[permT, xn], W=[pr])
                    mk.do("gpsimd", lambda e: e.tensor_tensor(out=t1[:, 0:n], in0=xn[:, 0:n], in1=cs[:, 0:n], op=ALU.mult), R=[xn, cs], W=[t1])
                    mk.do("vector", lambda e: e.tensor_tensor(out=xn[:, 0:n], in0=pr[:, 0:n], in1=sn[:, 0:n], op=ALU.mult), R=[pr, sn], W=[xn])
                    ob = obp.next()
                    mk.do("gpsimd", lambda e: e.tensor_tensor(out=ob[:, 0:n], in0=t1[:, 0:n], in1=xn[:, 0:n], op=ALU.add), R=[t1, xn], W=[ob])
                    mk.dma("sync", dst[:, tb:tb + n], ob[:, 0:n], R=[ob])

                for ch in range(4):
                    p = proj(lambda k: winb[:, k, ch * 128:(ch + 1) * 128])
                    norm_rope(p, 0, QC[ch])
                for h in range(2):
                    p = proj(lambda k: wkd[:, k, h, :])
                    norm_rope(p, 1, KC[h])
                for ch in range(4):
                    for (c0, dst) in ((768, QD), (1280, KD)):
                        p = proj(lambda k: winb[:, k, c0 + ch * 128:c0 + (ch + 1) * 128])
                        ob = obp.next()
                        mk.do("scalar", lambda e: e.activation(out=ob[:, 0:n], in_=p[:, 0:n], func=AF.Copy), R=[p], W=[ob])
                        mk.dma("sync", dst[ch, :, tb:tb + n], ob[:, 0:n], R=[ob])
                for j in range(n // 128):
                    pv = g.pM.next()
                    for k in range(8):
                        mk.do("tensor", lambda e: e.matmul(pv[:, 0:128], hT[:, k, j * 128:(j + 1) * 128], winb[:, k, 640:768], start=(k == 0), stop=(k == 7)),
                              R=[winb, hT], W=[pv])
                    vo = vcp.next()
                    mk.do("vector", lambda e: e.memset(vo[:, :, 64:65], 1.0), W=[vo])
                    mk.do("vector", lambda e: e.tensor_copy(out=vo[:, :, 0:64], in_=pv[:, 0:128].rearrange("p (h d) -> p h d", h=2)), R=[pv], W=[vo], join=True)
                    mk.dma("sync", VC[tb + j * 128:tb + (j + 1) * 128, :, :], vo[:, :, :], R=[vo])
                    pv2 = g.pM.next()
                    for k in range(8):
                        mk.do("tensor", lambda e: e.matmul(pv2[:, 0:512], hT[:, k, j * 128:(j + 1) * 128], winb[:, k, 1792:2304], start=(k == 0), stop=(k == 7)),
                              R=[winb, hT], W=[pv2])
                    vo2 = vdp.next()
                    mk.do("gpsimd", lambda e: e.memset(vo2[:, :, 64:65], 1.0), W=[vo2])
                    mk.do("scalar", lambda e: e.activation(out=vo2[:, :, 0:64], in_=pv2[:, 0:512].rearrange("p (h d) -> p h d", h=8), func=AF.Copy), R=[pv2], W=[vo2], join=True)
                    mk.dma("sync", VD[tb + j * 128:tb + (j + 1) * 128, :, :], vo2[:, :, :], R=[vo2])
        mk.barrier()

        def finish_head(po, n, ph, dst_ap, pools):
            dtmp, bsb, ot = pools["dtmp"].next(), pools["bsb"].next(), pools["ot"].next()
            mk.do("vector", lambda e: e.reciprocal(out=dtmp[64:65, 0:n], in_=po[64:65, 0:n]), R=[po], W=[dtmp])
            pb = g.pT.next()
            mk.do("tensor", lambda e: e.matmul(pb[0:64, 0:n], g.ones[64:65, 0:64], dtmp[64:65, 0:n], start=True, stop=True), R=[g.ones, dtmp], W=[pb])
            mk.do("scalar", lambda e: e.activation(out=bsb[0:64, 0:n], in_=pb[0:64, 0:n], func=AF.Copy), R=[pb], W=[bsb])
            mk.do("vector", lambda e: e.tensor_tensor(out=ot[ph * 64:(ph + 1) * 64, 0:n], in0=po[0:64, 0:n], in1=bsb[0:64, 0:n], op=ALU.mult), R=[po, bsb], W=[ot])
            mk.dma("sync", dst_ap, ot[ph * 64:(ph + 1) * 64, 0:n], R=[ot])

        with mk.scope():
            bint = mk.sb("bint", [128, 12, 512], F32)
            bedge = mk.sb("bedge", [128, 12, 512], F32)
            qdp = mk.pool("qdt", [128, 512], BF16, 2)
            kdp = mk.pool("kdt", [128, 14 * 128], BF16, 2)
            vdp2 = mk.pool("vdt", [128, 14, 65], BF16, 2)
            tmpp = mk.pool("natmp", [128, 512], F32, 2)
            ptp = mk.pool("napt", [128, 512], BF16, 3)
            pools = {"dtmp": mk.pool("dtmp", [128, 512], F32, 2), "bsb": mk.pool("bsb", [128, 512], F32, 2), "ot": mk.pool("ot", [128, 512], BF16, 2)}
            nabr = nab.rearrange("(v h c) p q -> v h p c q", v=3, h=8)
            for h in range(8):
                ph, ch = h % 2, h // 2
                hs = slice(ph * 64, (ph + 1) * 64)
                mk.dma("sync", bint[:], nabr[0, h], W=[bint])
                for qb in range(NQB + 1):
                    isctx = qb == NQB
                    n = 256 if isctx else 512
                    t0 = NLAT if isctx else qb * 512
                    qt, kt, vt = qdp.next(), kdp.next(), vdp2.next()
                    mk.dma("sync", qt[hs, 0:n], QD[ch, hs, t0:t0 + n], W=[qt])
                    mk.dma("sync", kt[hs, 0:256], KD[ch, hs, NLAT:NLAT + 256], W=[kt])
                    mk.dma("sync", vt[:, 0:2, :], VD[NLAT:NLAT + 256, h, :].rearrange("(c p) d -> p c d", p=128), W=[vt], join=True)
                    chunks = [(0, None), (1, None)]
                    bias = bint
                    if not isctx:
                        r0 = qb * 8
                        cl = [c for c in range(12) if 0 <= (r0 - 8 + 2 * c) < ROWS]
                        c_lo, c_hi = cl[0], cl[-1] + 1
                        ts = (r0 - 8 + 2 * c_lo) * 64
                        mk.dma("sync", kt[hs, 256 + c_lo * 128:256 + c_hi * 128], KD[ch, hs, ts:ts + (c_hi - c_lo) * 128], W=[kt], join=True)
                        mk.dma("sync", vt[:, 2 + c_lo:2 + c_hi, :], VD[ts:ts + (c_hi - c_lo) * 128, h, :].rearrange("(c p) d -> p c d", p=128), W=[vt], join=True)
                        chunks += [(2 + c, c) for c in cl]
                        if qb == 0 or qb == NQB - 1:
                            mk.dma("sync", bedge[:], nabr[1 if qb == 0 else 2, h], W=[bedge])
                            bias = bedge
                    po = g.pY.next()
                    for ci, (idx, c) in enumerate(chunks):
                        ps = g.pM.next()
                        mk.do("tensor", lambda e: e.matmul(ps[:, 0:n], kt[hs, idx * 128:(idx + 1) * 128], qt[hs, 0:n], start=True, stop=True), R=[kt, qt], W=[ps])
                        pt = ptp.next()
                        if c is None:
                            mk.do("scalar", lambda e: e.activation(out=pt[:, 0:n], in_=ps[:, 0:n], func=AF.Exp, scale=0.125), R=[ps], W=[pt])
                        else:
                            tm = tmpp.next()
                            mk.do("vector", lambda e: e.scalar_tensor_tensor(out=tm[:, 0:n], in0=ps[:, 0:n], scalar=0.125, in1=bias[:, c, 0:n], op0=ALU.mult, op1=ALU.add),
                                  R=[ps, bias], W=[tm])
                            mk.do("scalar", lambda e: e.activation(out=pt[:, 0:n], in_=tm[:, 0:n], func=AF.Exp), R=[tm], W=[pt])
                        mk.do("tensor", lambda e: e.matmul(po[0:65, 0:n], vt[:, idx, 0:65], pt[:, 0:n], start=(ci == 0), stop=(ci == len(chunks) - 1)), R=[vt, pt], W=[po])
                    finish_head(po, n, ph, OT[4 + ch, hs, t0:t0 + n], pools)

        with mk.scope():
            NKC = NT // 128
            kcs = [mk.sb("kcs%d" % h, [128, NT], BF16) for h in range(2)]
            vcs = mk.sb("vcs", [128, NKC, 2, 65], BF16)
            for h in range(2):
                for i, c0 in enumerate(range(0, NT, 4096)):
                    c1 = min(NT, c0 + 4096)
                    mk.dma("sync", kcs[h][:, c0:c1], KC[h, :, c0:c1], W=[kcs[h]], join=True)
            for c0 in range(0, NKC, 26):
                c1 = min(NKC, c0 + 26)
                mk.dma("sync", vcs[:, c0:c1, :, :], VC[c0 * 128:c1 * 128, :, :].rearrange("(c p) h d -> p c h d", p=128), W=[vcs], join=True)
            qcp = mk.pool("qct", [128, 512], BF16, 2)
            ptp = mk.pool("gpt", [128, 512], BF16, 4)
            pools = {"dtmp": mk.pool("dtmp", [128, 512], F32, 2), "bsb": mk.pool("bsb", [128, 512], F32, 2), "ot": mk.pool("ot", [128, 512], BF16, 2)}
            for h in range(8):
                ph, ch, kvh = h % 2, h // 2, h // 4
                hs = slice(ph * 64, (ph + 1) * 64)
                for qb in range(NQB + 1):
                    isctx = qb == NQB
                    n = 256 if isctx else 512
                    t0 = NLAT if isctx else qb * 512
                    qt = qcp.next()
                    mk.dma("sync", qt[hs, 0:n], QC[ch, hs, t0:t0 + n], W=[qt])
                    kchunks = list(range(NLAT // 128, NKC)) if isctx else list(range(NKC))
                    po = g.pY.next()
                    for ci, kc in enumerate(kchunks):
                        ps = g.pM.next()
                        mk.do("tensor", lambda e: e.matmul(ps[:, 0:n], kcs[kvh][hs, kc * 128:(kc + 1) * 128], qt[hs, 0:n], start=True, stop=True), R=[kcs[kvh], qt], W=[ps])
                        pt = ptp.next()
                        mk.do("scalar", lambda e: e.activation(out=pt[:, 0:n], in_=ps[:, 0:n], func=AF.Exp, scale=0.125), R=[ps], W=[pt])
                        mk.do("tensor", lambda e: e.matmul(po[0:65, 0:n], vcs[:, kc, kvh, 0:65], pt[:, 0:n], start=(ci == 0), stop=(ci == len(kchunks) - 1)), R=[vcs, pt], W=[po])
                    finish_head(po, n, ph, OT[ch, hs, t0:t0 + n], pools)
        mk.barrier()

        with mk.scope():
            woutb = mk.sb("woutb", [128, 8, D], BF16)
            load_cast(g, woutb, w_out, 8, D)
            lnG = mk.sb("ln1g", [128, D], F32)
            lnB = mk.sb("ln1b", [128, D], F32)
            load_bcast(g, lnG, g.ln[0:1, :])
            load_bcast(g, lnB, g.ln[1:2, :])
            catp = mk.pool("cat", [128, 8, 128], BF16, 3)
            tp = {"r": mk.pool("lr", [128, D], F32, 2), "r2": mk.pool("lr2", [128, D], F32, 2), "st": mk.pool("lst", [128, 8], F32, 2)}
            for (s, t0, n) in blocks:
                for j in range(n // 128):
                    row = tokbase(s, t0) + j * 128
                    cat = catp.next()
                    mk.dma("sync", cat[:, :, :], OT[:, :, row:row + 128].rearrange("c p t -> p c t"), W=[cat])
                    pys = []
                    for half in range(2):
                        py = g.pY.next()
                        for kc in range(8):
                            mk.do("tensor", lambda e: e.matmul(py[:], cat[:, kc, :], woutb[:, kc, half * 512:(half + 1) * 512], start=(kc == 0), stop=(kc == 7)),
                                  R=[cat, woutb], W=[py])
                        pys.append(py)
                    xt = g.xt.next()
                    mk.dma("sync", xt[:], tok_src(g, s, t0 + j * 128, 128), W=[xt])
                    ln_epilogue(g, [pys[0][:], pys[1][:]], pys, xt, gB[(2, s)], lnG, lnB, g.x1[row:row + 128, :], tp)
        mk.barrier()
        mk.es = old_es
        es_g1.close()
        if not DEBUG.get("skip_moe"):
            moe_phase(g, {0: gB[(5, 0)], 1: gB[(5, 1)]})
        mk.barrier()
        print("odd: nsem", mk.nsem, {k: v.ninst for k, v in mk.E.items()})
        if not standalone:
            mk.finish()
    return nc


def odd_maps(maps, xl, xc, w_in, w_out, qg, kg, rel_bias):
    NT = NLAT + NCTX
    ROWS = NLAT // 64
    t = np.arange(NLAT)
    row, col = (t // 64).astype(np.float64), (t % 64).astype(np.float64)
    inv = 10000.0 ** (-np.arange(0, 32, 2, dtype=np.float64) / 32)
    cos = np.ones((128, NT), np.float32)
    sin = np.zeros((128, NT), np.float32)
    permT = np.zeros((128, 128), np.float32)
    for p in range(128):
        d = p % 64
        dd = d % 32
        pos = row if d < 32 else col
        ang = pos * inv[dd % 16]
        cos[p, :NLAT] = np.cos(ang)
        sin[p, :NLAT] = (-1.0 if dd < 16 else 1.0) * np.sin(ang)
        partner = p + 16 if dd < 16 else p - 16
        permT[partner, p] = 1.0
    blk = np.zeros((128, 128), np.float32)
    blk[0:64, 0:64] = 1.0 / 64
    blk[64:128, 64:128] = 1.0 / 64
    qkg = np.stack([np.tile(qg, 2), np.tile(kg, 2)], axis=1).astype(np.float32)
    nab = np.full((3, 8, 12, 2, 64, 8, 64), NEG, np.float32)
    kc = np.arange(64)[:, None]
    qc = np.arange(64)[None, :]
    cstart = np.clip(qc - 8, 0, 48)
    colok = (kc >= cstart) & (kc < cstart + 16)
    cidx = np.clip(kc - qc + 15, 0, 30)
    for v, r0 in enumerate((8, 0, ROWS - 8)):
        for c in range(12):
            for kr in range(2):
                krow = r0 - 8 + 2 * c + kr
                if not (0 <= krow < ROWS):
                    continue
                for qr in range(8):
                    qrow = r0 + qr
                    rs = min(max(qrow - 4, 0), ROWS - 8)
                    if not (rs <= krow < rs + 8):
                        continue
                    vals = rel_bias[:, krow - qrow + 7, :][:, cidx]
                    nab[v, :, c, kr, :, qr, :] = np.where(colok[None], vals, NEG)
    nab = np.ascontiguousarray(nab.reshape(3 * 8 * 12, 128, 512))
    for core in range(NCORES):
        maps[core].update({"w_in": w_in, "w_out": w_out, "ropec": cos, "ropes": sin, "permT": permT, "blk64": blk,
                           "qkg": qkg, "nab": nab})
```
